# Optimizing a Trainium2 kernel written in Bass

```python
import jax, jax.numpy as jnp
from jax import lax
import numpy as np

D_MODEL = 1024
BATCH = 8
SEQ = 4096
DEPTH = 4

CHUNK = 64
N_MIXERS = 2
N_A_LAYERS = (DEPTH + 1) // 2
N_B_LAYERS = DEPTH // 2
N_VRES = max(N_B_LAYERS - 1, 0)
PLE_DIM = 256
D_FF = 2816
RMS_EPS = 1e-6
GMLP_BLOCK = 128
GMLP_WIDTH = 2 * D_MODEL
GMLP_GROUPS = 8
GMLP_GROUP_DIM = GMLP_WIDTH // GMLP_GROUPS
LN_EPS = 1e-5
RWKV_HEAD_DIM = 64
RWKV_HEADS = D_MODEL // RWKV_HEAD_DIM
DECAY_LORA = 64
AAA_LORA = 64
MV_LORA = 32
GATE_LORA = 160
GN_EPS = 64e-5

kernel_name = "hybrid_gmlp_rwkv7_macaron_trunk"


def rms_norm(x, g):
    xf = x.astype(jnp.float32)
    y = xf * lax.rsqrt(jnp.mean(xf * xf, axis=-1, keepdims=True) + RMS_EPS)
    return (y * g.astype(jnp.float32)).astype(x.dtype)


def swiglu(x, w13, w2):
    gate, up = jnp.split(x @ w13, 2, axis=-1)
    return (jax.nn.silu(gate) * up) @ w2


def gmlp_mixer(h, w_in, b_in, v_gain, w_s, b_s, w_out):
    B, S, _ = h.shape
    z = jax.nn.gelu(h @ w_in + b_in)
    u, v = jnp.split(z, 2, axis=-1)
    vf = v.astype(jnp.float32)
    mean = jnp.mean(vf, axis=-1, keepdims=True)
    var = jnp.mean(jnp.square(vf - mean), axis=-1, keepdims=True)
    v = ((vf - mean) * lax.rsqrt(var + LN_EPS) * v_gain.astype(jnp.float32)).astype(h.dtype)
    nb = S // GMLP_BLOCK
    v = v.reshape(B, nb, GMLP_BLOCK, GMLP_GROUPS, GMLP_GROUP_DIM)
    u = u.reshape(B, nb, GMLP_BLOCK, GMLP_GROUPS, GMLP_GROUP_DIM)
    cidx = jnp.arange(GMLP_BLOCK) // CHUNK
    mask = cidx[None, :] <= cidx[:, None]
    ws = jnp.where(mask[None], w_s, jnp.zeros_like(w_s))
    s = jnp.einsum('gij,bcjgd->bcigd', ws, v) + b_s.T[None, None, :, :, None]
    y = (u * s).reshape(B, S, GMLP_WIDTH)
    return y @ w_out


def wkv7_scan(r, w, k, v, a, b):
    B, S, H, N = r.shape

    def step(state, inp):
        r_t, w_t, k_t, v_t, a_t, b_t = inp
        sa = jnp.einsum('bhij,bhj->bhi', state, a_t)
        state = (state * w_t[:, :, None, :] + sa[..., None] * b_t[:, :, None, :]
                 + v_t[..., None] * k_t[:, :, None, :])
        y_t = jnp.einsum('bhij,bhj->bhi', state, r_t)
        return state, y_t

    xs = tuple(jnp.moveaxis(t, 1, 0) for t in (r, w, k, v, a, b))
    state0 = jnp.zeros((B, H, N, N), jnp.float32)
    _, ys = lax.scan(step, state0, xs)
    return jnp.moveaxis(ys, 0, 1)


def rwkv7_mixer(h, v_first, vres, mu, w_in, w0, w1, w2, a0, a1, a2, g1, g2,
                k_k, k_a, r_k, lnx, w_out):
    B, S, D = h.shape
    H, N = RWKV_HEADS, RWKV_HEAD_DIM
    f32 = jnp.float32
    dx = jnp.pad(h, ((0, 0), (1, 0), (0, 0)))[:, :-1] - h
    x_rkv = h[:, :, None, :] + dx[:, :, None, :] * mu[:3]
    rkv = jnp.einsum('bsnd,nde->bsne', x_rkv, w_in)
    r, k, v = rkv[:, :, 0], rkv[:, :, 1], rkv[:, :, 2]
    xw = h + dx * mu[3]
    xa = h + dx * mu[4]
    xg = h + dx * mu[5]
    zw = (w0 + jnp.tanh(xw @ w1) @ w2).astype(f32)
    decay = jnp.exp(-jnp.exp(-jax.nn.softplus(-zw) - 0.5))
    a = jax.nn.sigmoid(a0 + (xa @ a1) @ a2)
    g = jax.nn.sigmoid(xg @ g1) @ g2

    def heads(t):
        return t.reshape(B, S, H, N).astype(f32)

    kk = heads(k * k_k)
    kk = kk / jnp.maximum(jnp.sqrt(jnp.sum(kk * kk, axis=-1, keepdims=True)), 1e-12)
    k = k * (1.0 + (a - 1.0) * k_a)
    if vres is None:
        v_first = v
    else:
        v0, v1, v2 = vres
        xv = h + dx * mu[2]
        v = v + (v_first - v) * jax.nn.sigmoid(v0 + (xv @ v1) @ v2)
    rh, kh, vh, ah = heads(r), heads(k), heads(v), heads(a)
    y = wkv7_scan(rh, heads(decay), kh, vh, -kk, kk * ah)
    mean = jnp.mean(y, axis=-1, keepdims=True)
    var = jnp.mean(jnp.square(y - mean), axis=-1, keepdims=True)
    yn = ((y - mean) * lax.rsqrt(var + GN_EPS)).reshape(B, S, D)
    yn = yn * lnx[0].astype(f32) + lnx[1].astype(f32)
    bonus = (jnp.sum(rh * kh * r_k.astype(f32), axis=-1, keepdims=True) * vh).reshape(B, S, D)
    out = ((yn + bonus).astype(h.dtype) * g) @ w_out
    return out, v_first


def setup_inputs(seed: int = 0) -> dict:
    key = jax.random.key(seed)
    ks = iter(jax.random.split(key, 40))
    f32 = jnp.float32

    def nrm(shape, scale):
        return jax.random.normal(next(ks), shape, f32) * scale

    def gain(shape):
        return 1.0 + nrm(shape, 0.05)

    D, E2, E = D_MODEL, 2 * GMLP_WIDTH, GMLP_WIDTH
    NA, NB = N_A_LAYERS, N_B_LAYERS
    return {
        "x": nrm((BATCH, SEQ, D), 1.0),
        "p": nrm((DEPTH, BATCH, SEQ, PLE_DIM), 1.0),
        "norm_g": gain((DEPTH, 8, D)),
        "ffn_w13": nrm((DEPTH, 2, D, 2 * D_FF), D ** -0.5),
        "ffn_w2": nrm((DEPTH, 2, D_FF, D), D_FF ** -0.5),
        "ple_w_gate": nrm((DEPTH, D, D), D ** -0.5),
        "ple_w_proj": nrm((DEPTH, PLE_DIM, D), PLE_DIM ** -0.5),
        "a_w_in": nrm((NA, D, E2), D ** -0.5),
        "a_b_in": nrm((NA, E2), 0.02),
        "a_v_gain": gain((NA, E)),
        "a_w_s": nrm((NA, GMLP_GROUPS, GMLP_BLOCK, GMLP_BLOCK), GMLP_BLOCK ** -0.5),
        "a_b_s": gain((NA, GMLP_GROUPS, GMLP_BLOCK)),
        "a_w_out": nrm((NA, E, D), E ** -0.5),
        "b_mu": jax.random.uniform(next(ks), (NB, 6, D), f32),
        "b_w_in": nrm((NB, 3, D, D), D ** -0.5),
        "b_w0": nrm((NB, D), 0.5),
        "b_w1": nrm((NB, D, DECAY_LORA), D ** -0.5),
        "b_w2": nrm((NB, DECAY_LORA, D), DECAY_LORA ** -0.5),
        "b_a0": nrm((NB, D), 0.1),
        "b_a1": nrm((NB, D, AAA_LORA), D ** -0.5),
        "b_a2": nrm((NB, AAA_LORA, D), AAA_LORA ** -0.5),
        "b_g1": nrm((NB, D, GATE_LORA), D ** -0.5),
        "b_g2": nrm((NB, GATE_LORA, D), GATE_LORA ** -0.5),
        "b_k_k": 0.85 + nrm((NB, D), 0.05),
        "b_k_a": gain((NB, D)),
        "b_r_k": nrm((NB, RWKV_HEADS, RWKV_HEAD_DIM), 0.1),
        "b_lnx": jnp.stack([gain((NB, D)), nrm((NB, D), 0.02)], axis=1),
        "b_w_out": nrm((NB, D, D), D ** -0.5),
        "b_v0": nrm((N_VRES, D), 0.1),
        "b_v1": nrm((N_VRES, D, MV_LORA), D ** -0.5),
        "b_v2": nrm((N_VRES, MV_LORA, D), MV_LORA ** -0.5),
    }


def reference(x, p, norm_g, ffn_w13, ffn_w2, ple_w_gate, ple_w_proj,
              a_w_in, a_b_in, a_v_gain, a_w_s, a_b_s, a_w_out,
              b_mu, b_w_in, b_w0, b_w1, b_w2, b_a0, b_a1, b_a2, b_g1, b_g2,
              b_k_k, b_k_a, b_r_k, b_lnx, b_w_out, b_v0, b_v1, b_v2):
    h = x
    v_first = None
    for i in range(DEPTH):
        g = norm_g[i]
        h = h + 0.5 * rms_norm(swiglu(rms_norm(h, g[0]), ffn_w13[i, 0], ffn_w2[i, 0]), g[1])
        hn = rms_norm(h, g[2])
        j = i // N_MIXERS
        if i % N_MIXERS == 0:
            m = gmlp_mixer(hn, a_w_in[j], a_b_in[j], a_v_gain[j], a_w_s[j], a_b_s[j], a_w_out[j])
        else:
            vres = None if v_first is None else (b_v0[j - 1], b_v1[j - 1], b_v2[j - 1])
            m, v_first = rwkv7_mixer(hn, v_first, vres, b_mu[j], b_w_in[j], b_w0[j], b_w1[j],
                                     b_w2[j], b_a0[j], b_a1[j], b_a2[j], b_g1[j], b_g2[j],
                                     b_k_k[j], b_k_a[j], b_r_k[j], b_lnx[j], b_w_out[j])
        h = h + rms_norm(m, g[3])
        h = h + 0.5 * rms_norm(swiglu(rms_norm(h, g[4]), ffn_w13[i, 1], ffn_w2[i, 1]), g[5])
        gate = jax.nn.sigmoid(rms_norm(h, g[6]) @ ple_w_gate[i])
        h = h + rms_norm(gate * (p[i] @ ple_w_proj[i]), g[7])
    return h
```

```python
import math
from contextlib import ExitStack

import numpy as np
import concourse.bass as bass
import concourse.mybir as mybir
from concourse.bass_utils import run_bass_kernel_spmd

F32 = mybir.dt.float32
BF16 = mybir.dt.bfloat16
AF = mybir.ActivationFunctionType
ALU = mybir.AluOpType
AX = mybir.AxisListType

D = 1024
NCH = 8
TT = 512
SEQ = 4096
DFF = 2816
NFC = 22
SLOT = 4096
RING = 5
C0 = math.exp(-0.5)


class T:
    __slots__ = ("w", "r")

    def __init__(self):
        self.w = None
        self.r = {}


class V:
    __slots__ = ("ap", "ts")

    def __init__(self, ap, ts):
        self.ap = ap
        self.ts = ts

    def __getitem__(self, idx):
        return V(self.ap[idx], self.ts)

    def re(self, pat, **kw):
        return V(self.ap.rearrange(pat, **kw), self.ts)

    def bc(self, shape):
        return V(self.ap.broadcast_to(list(shape)), self.ts)


class Eng:
    def __init__(self, name, same_wait):
        self.name = name
        self.items = []
        self.count = 0
        self.seen = {}
        self.same_wait = same_wait
        self.semkey = "E_" + name


class FW:
    def __init__(self, nc, stack):
        self.nc = nc
        self.stack = stack
        self.sems = {}
        self.engs = {}
        for name, sw in (("pe", False), ("act", True), ("dve", True), ("pool", True), ("sp", False)):
            e = Eng(name, sw)
            self.engs[name] = e
            self.sems[e.semkey] = stack.enter_context(nc.semaphore("s_" + name))
        self.dma_cnt = {}
        self.n_ins = 0

    def new_sem(self, key):
        self.sems[key] = self.stack.enter_context(self.nc.semaphore(key))
        self.dma_cnt[key] = 0
        return key

    def _waits(self, eng, reads, writes):
        need = {}

        def add(ev):
            if ev is None:
                return
            k, v = ev
            if k == eng.semkey and not eng.same_wait:
                return
            if eng.seen.get(k, 0) >= v:
                return
            if need.get(k, 0) < v:
                need[k] = v

        for t in reads:
            add(t.w)
        for t in writes:
            add(t.w)
            for k, v in t.r.items():
                add((k, v))
        for k, v in need.items():
            eng.seen[k] = v
            h = self.sems[k]
            if k == eng.semkey:
                assert v <= eng.count, "self-wait on pending event"
            eng.items.append(("w", h, v))

    def op(self, engname, meth, args, kw, reads, writes, inc=True):
        eng = self.engs[engname]
        self._waits(eng, reads, writes)
        self.n_ins += 1
        if inc:
            eng.count += 1
            eng.items.append(("i", meth, args, kw, self.sems[eng.semkey], 1))
            ev = (eng.semkey, eng.count)
        else:
            eng.items.append(("i", meth, args, kw, None, 0))
            ev = (eng.semkey, eng.count + 1)
        for t in reads:
            if t.r.get(ev[0], 0) < ev[1]:
                t.r[ev[0]] = ev[1]
        for t in writes:
            t.w = ev
            t.r = {}
        return ev

    def dma(self, engname, semkey, out, in_, reads=(), writes=()):
        eng = self.engs[engname]
        self._waits(eng, reads, writes)
        self.n_ins += 1
        self.dma_cnt[semkey] += 16
        eng.items.append(("i", "dma_start", (), {"out": out, "in_": in_}, self.sems[semkey], 16))
        ev = (semkey, self.dma_cnt[semkey])
        for t in reads:
            if t.r.get(ev[0], 0) < ev[1]:
                t.r[ev[0]] = ev[1]
        for t in writes:
            t.w = ev
            t.r = {}
        return ev

    def wait_all(self, engname, tiles):
        self._waits(self.engs[engname], (), tiles)

    def emit(self):
        def run(e, items):
            for it in items:
                if it[0] == "w":
                    e.wait_ge(it[1], it[2])
                else:
                    _, meth, args, kw, sem, n = it
                    ins = getattr(e, meth)(*args, **kw)
                    if sem is not None:
                        ins.then_inc(sem, n)

        with self.nc.Block() as block:
            @block.tensor
            def _(e):
                run(e, self.engs["pe"].items)

            @block.scalar
            def _(e):
                run(e, self.engs["act"].items)

            @block.vector
            def _(e):
                run(e, self.engs["dve"].items)

            @block.gpsimd
            def _(e):
                run(e, self.engs["pool"].items)

            @block.sync
            def _(e):
                run(e, self.engs["sp"].items)


def _kmaj(w):
    K, N = w.shape
    return w.reshape(K // 128, 128, N).transpose(1, 0, 2)


def _fm(v):
    return np.asarray(v, np.float32).reshape(-1, 128).T


def _pad_rows(a):
    out = np.zeros((128,) + a.shape[1:], np.float32)
    out[: a.shape[0]] = a
    return out


def block_list(inp, nlayers=4):
    blocks = []

    def add(name, arr):
        a = np.ascontiguousarray(np.asarray(arr, np.float32).reshape(128, -1))
        assert a.shape[1] <= SLOT, (name, a.shape)
        blocks.append((name, a))

    def ffn(l, i):
        w13k = _kmaj(inp["ffn_w13"][l, i])
        for b in range(11):
            g = w13k[:, :, 256 * b:256 * b + 256]
            u = w13k[:, :, DFF + 256 * b:DFF + 256 * b + 256]
            add(f"L{l}.f{i}.w13.{b}", np.stack([g, u], axis=2))
        w2k = _kmaj(inp["ffn_w2"][l, i])
        for c in range(8):
            add(f"L{l}.f{i}.w2.{c}", w2k[:, :, 128 * c:128 * c + 128])

    for l in range(nlayers):
        ffn(l, 0)
        j = l // 2
        if l % 2 == 0:
            win = _kmaj(inp["a_w_in"][j])
            for q in range(4):
                add(f"L{l}.g.wu.{q}", win[:, :, 512 * q:512 * q + 512])
            rows = np.zeros((128, 3072), np.float32)
            rows[0, :2048] = inp["a_b_in"][j][2048:]
            rows[0, 2048:] = inp["a_b_s"][j].reshape(-1)
            add(f"L{l}.g.rows", rows)
            for q in range(4):
                add(f"L{l}.g.wv.{q}", win[:, :, 2048 + 512 * q:2048 + 512 * q + 512])
            add(f"L{l}.g.vg", np.broadcast_to(inp["a_v_gain"][j][None, :], (128, 2048)))
            add(f"L{l}.g.ws", inp["a_w_s"][j].transpose(2, 0, 1))
            wo = _kmaj(inp["a_w_out"][j])
            for q in range(4):
                add(f"L{l}.g.wo.{q}", wo[:, :, 256 * q:256 * q + 256])
        else:
            la = np.concatenate([
                _kmaj(inp["b_w1"][j]).reshape(128, -1), _kmaj(inp["b_a1"][j]).reshape(128, -1),
                _kmaj(inp["b_g1"][j]).reshape(128, -1),
                (_kmaj(inp["b_v1"][j - 1]).reshape(128, -1) if j >= 1 else np.zeros((128, 256), np.float32))],
                axis=1)
            lb = np.concatenate([
                _pad_rows(inp["b_w2"][j]), _pad_rows(inp["b_a2"][j]),
                (_pad_rows(inp["b_v2"][j - 1]) if j >= 1 else np.zeros((128, 1024), np.float32))], axis=1)
            lc = np.concatenate([inp["b_g2"][j][:128], _pad_rows(inp["b_g2"][j][128:])], axis=1)
            wks = [_kmaj(inp["b_w_in"][j, n]) for n in range(3)]
            wo = _kmaj(inp["b_w_out"][j])
            for s in range(2):
                add(f"L{l}.r.la.{s}", la)
                add(f"L{l}.r.lb.{s}", lb)
                for n in (1, 0, 2):
                    for q in range(2):
                        add(f"L{l}.r.win{n}.{q}.{s}", wks[n][:, :, 512 * q:512 * q + 512])
                add(f"L{l}.r.lc.{s}", lc)
                for q in range(2):
                    add(f"L{l}.r.wo.{q}.{s}", wo[:, :, 512 * q:512 * q + 512])
        ffn(l, 1)
        add(f"L{l}.p.wp", _kmaj(inp["ple_w_proj"][l]))
        wg = _kmaj(inp["ple_w_gate"][l])
        for q in range(2):
            add(f"L{l}.p.wg.{q}", wg[:, :, 512 * q:512 * q + 512])
    return blocks


def p1_layout():
    cols = {}
    n = 0

    def add(name, k):
        nonlocal n
        cols[name] = n
        n += k

    add("g", 4 * 8 * 8)
    for j in range(2):
        add(f"binu{j}", 16)
    for j in range(2):
        add(f"mu{j}", 48)
        for nm in ("w0", "a0", "kk", "ka", "rk", "ln0", "ln1", "v0", "omka"):
            add(f"{nm}{j}", 8)
    for nm in ("eps_rms", "eps_rms4", "eps_ln", "eps_gn", "tiny"):
        add(nm, 1)
    return cols, n


def build_p1(inp):
    cols, n = p1_layout()
    p = np.zeros((128, n), np.float32)
    for l in range(4):
        for i in range(8):
            c = cols["g"] + (l * 8 + i) * 8
            p[:, c:c + 8] = _fm(inp["norm_g"][l, i])
    for j in range(2):
        p[:, cols[f"binu{j}"]:cols[f"binu{j}"] + 16] = _fm(inp["a_b_in"][j][:2048])
        for m in range(6):
            c = cols[f"mu{j}"] + m * 8
            p[:, c:c + 8] = _fm(inp["b_mu"][j, m])
        for nm, key in (("w0", "b_w0"), ("a0", "b_a0"), ("kk", "b_k_k"), ("ka", "b_k_a")):
            p[:, cols[f"{nm}{j}"]:cols[f"{nm}{j}"] + 8] = _fm(inp[key][j])
        p[:, cols[f"rk{j}"]:cols[f"rk{j}"] + 8] = _fm(inp["b_r_k"][j].reshape(-1))
        p[:, cols[f"ln0{j}"]:cols[f"ln0{j}"] + 8] = _fm(inp["b_lnx"][j, 0])
        p[:, cols[f"ln1{j}"]:cols[f"ln1{j}"] + 8] = _fm(inp["b_lnx"][j, 1])
        if j >= 1:
            p[:, cols[f"v0{j}"]:cols[f"v0{j}"] + 8] = _fm(inp["b_v0"][j - 1])
    p[:, cols["eps_rms"]] = 1e-6
    p[:, cols["eps_rms4"]] = 4e-6
    p[:, cols["eps_ln"]] = 1e-5
    p[:, cols["eps_gn"]] = 64e-5
    p[:, cols["tiny"]] = 1e-24
    return p


CST = {}


def cst_layout():
    cols = {}
    n = 0
    for name, k in (("ones_mean", 128), ("ones_q", 128), ("blk_gn", 128), ("blk_1", 128), ("ident", 128),
                    ("ones1", 128), ("maskg4", 512), ("maskn16", 1024), ("ident16", 1024)):
        cols[name] = n
        n += k
    return cols, n


def build_cst():
    cols, n = cst_layout()
    c = np.zeros((128, n), np.float32)
    c[:, cols["ones_mean"]:cols["ones_mean"] + 128] = 1.0 / 1024
    c[:, cols["ones_q"]:cols["ones_q"] + 128] = 1.0 / 256
    blk = np.zeros((128, 128), np.float32)
    blk[:64, :64] = 1
    blk[64:, 64:] = 1
    c[:, cols["blk_gn"]:cols["blk_gn"] + 128] = blk / 64
    c[:, cols["blk_1"]:cols["blk_1"] + 128] = blk
    c[:, cols["ident"]:cols["ident"] + 128] = np.eye(128)
    c[:, cols["ones1"]:cols["ones1"] + 128] = 1.0
    s = np.arange(64)
    strict = (s[:, None] < s[None, :]).astype(np.float32)
    incl = (s[:, None] <= s[None, :]).astype(np.float32)
    mg = np.concatenate([np.concatenate([strict, incl], 1)] * 2, 0)
    c[:, cols["maskg4"]:cols["maskg4"] + 512] = np.tile(mg, (1, 4))
    mn = (s[:, None] > s[None, :]).astype(np.float32)
    c[:64, cols["maskn16"]:cols["maskn16"] + 1024] = np.tile(mn, (1, 16))
    c[:64, cols["ident16"]:cols["ident16"] + 1024] = np.tile(np.eye(64, dtype=np.float32), (1, 16))
    return c


class Role:
    def __init__(self, g, u0, nu, dt):
        self.g = g
        self.u0 = u0
        self.nu = nu
        base = g.arena[:, u0 * 512:(u0 + nu) * 512]
        self.ap = base if dt == BF16 else base.bitcast(F32)
        self.epu = 512 if dt == BF16 else 256
        self.n = nu * self.epu

    def v(self, c0=0, c1=None):
        c1 = self.n if c1 is None else c1
        us = range(self.u0 + c0 // self.epu, self.u0 + (c1 - 1) // self.epu + 1)
        return V(self.ap[:, c0:c1], [self.g.units[u] for u in us])

    def chunks(self, n, w):
        return [self.v(i * w, (i + 1) * w) for i in range(n)]


class WStream:
    def __init__(self, g, names, offs, ntiles):
        self.g = g
        self.names = names
        self.offs = offs
        self.total = len(names) * ntiles
        self.issued = 0
        self.pos = 0
        self.slots = []
        for i in range(RING):
            t = g.stack.enter_context(g.nc.sbuf_tensor(f"ring{i}", [128, SLOT], BF16))
            self.slots.append(V(t[:], [T()]))
            g.fw.new_sem(f"ring{i}")
        self.free = list(range(RING))
        self.loaded = {}
        self.held = {}
        self.last = None

    def pump(self):
        while self.free and self.issued < self.total:
            s = self.free.pop(0)
            name = self.names[self.issued % len(self.names)]
            off, n = self.offs[name]
            g = self.g
            g.fw.dma("sp", f"ring{s}", self.slots[s].ap[:, 0:n], g.Wb[:, off:off + n],
                     reads=[g.wbT[name]], writes=self.slots[s].ts)
            self.loaded[self.issued] = s
            self.issued += 1

    def get(self, name, hold=False):
        if self.last is not None:
            self.free.append(self.last)
            self.last = None
        self.pump()
        want = self.names[self.pos % len(self.names)]
        assert want == name, (want, name)
        s = self.loaded.pop(self.pos)
        self.pos += 1
        if hold:
            self.held[name] = s
        else:
            self.last = s
        return self.slots[s][:, 0:self.offs[name][1]]

    def release(self, name):
        self.free.append(self.held.pop(name))
        self.pump()


class Gen:
    def __init__(self, nc, stack, blocks_meta, ntiles, nlayers, s_core, debug_out=None):
        self.nc = nc
        self.stack = stack
        self.fw = FW(nc, stack)
        self.ntiles = ntiles
        self.nlayers = nlayers
        self.s_core = s_core
        names = [n for n, _ in blocks_meta]
        self.offs = {}
        off = 0
        for n, k in blocks_meta:
            self.offs[n] = (off, k)
            off += k
        self.ncols = off
        self.names = names
        self.p1c, self.np1 = p1_layout()
        self.cc, self.ncst = cst_layout()
        self.xT = nc.dram_tensor("xT", [128, NCH, s_core], F32, kind="ExternalInput").ap()
        self.pT = nc.dram_tensor("pT", [128, 4, 2, s_core], F32, kind="ExternalInput").ap()
        self.W = nc.dram_tensor("W", [128, self.ncols], F32, kind="ExternalInput").ap()
        self.P1d = nc.dram_tensor("P1", [128, self.np1], F32, kind="ExternalInput").ap()
        self.CSTd = nc.dram_tensor("CST", [128, self.ncst], F32, kind="ExternalInput").ap()
        self.yT = nc.dram_tensor("yT", [128, NCH, s_core], F32, kind="ExternalOutput").ap()
        self.Wb = nc.dram_tensor("Wb", [128, self.ncols], BF16, kind="Internal").ap()
        self.Pb = nc.dram_tensor("Pb", [128, 4, 2, s_core], BF16, kind="Internal").ap()
        self.wbT = {n: T() for n in names}
        self.pbT = T()

    def sb(self, name, shape, dt=F32):
        t = self.stack.enter_context(self.nc.sbuf_tensor(name, list(shape), dt))
        return V(t[:], [T()])

    def op(self, eng, meth, out, *args, inc=True, **kw):
        reads, writes = [], list(out.ts)
        a2 = [out.ap]
        for a in args:
            if isinstance(a, V):
                reads += a.ts
                a2.append(a.ap)
            else:
                a2.append(a)
        k2 = {}
        for k, v in kw.items():
            if isinstance(v, V):
                if k == "accum_out":
                    writes += v.ts
                else:
                    reads += v.ts
                k2[k] = v.ap
            else:
                k2[k] = v
        return self.fw.op(eng, meth, tuple(a2), k2, reads, writes, inc=inc)

    def mm(self, out, terms, tps=None):
        n = len(terms)
        for i, (l, r) in enumerate(terms):
            kw = {}
            if tps is not None and tps[i] is not None:
                kw["tile_position"] = tps[i]
            self.op("pe", "matmul", out, l, r, start=(i == 0), stop=(i == n - 1), inc=(i == n - 1), **kw)

    def bank(self):
        b = self.banks[self.bank_i]
        self.bank_i = (self.bank_i + 1) % 8
        return b

    def pc(self, name, k=0, n=1):
        c = self.p1c[name] + k
        return self.P1[:, c:c + n]

    def cm(self, name, r0=0, r1=128, c0=0, c1=128):
        c = self.cc[name]
        return self.CSTb[r0:r1, c + c0:c + c1]

    def setup(self):
        nc, st, fw = self.nc, self.stack, self.fw
        self.NU = 116
        at = st.enter_context(nc.sbuf_tensor("arena", [128, self.NU * 512], BF16))
        self.arena = at[:]
        self.units = [T() for _ in range(self.NU)]
        self.banks = []
        for i in range(8):
            t = st.enter_context(nc.psum_tensor(f"bank{i}", [128, 512], F32))
            self.banks.append(V(t[:], [T()]))
        self.bank_i = 0
        self.P1 = self.sb("P1s", [128, self.np1], F32)
        self.CSTb = self.sb("CSTs", [128, self.ncst], BF16)
        self.H = self.sb("H", [128, NCH * TT], F32)
        self.Hc = []
        ht = self.H.ap
        for k in range(NCH):
            self.Hc.append(V(ht[:, k * TT:(k + 1) * TT], [T()]))
        self.H.ts = [c.ts[0] for c in self.Hc]
        self.VF = self.sb("VF", [128, NCH * TT], BF16)
        self.VFc = [V(self.VF.ap[:, k * TT:(k + 1) * TT], [T()]) for k in range(NCH)]
        self.rstd = [self.sb(f"rstd{i}", [128, TT], F32) for i in range(2)]
        self.rstd_i = 0
        self.PT = self.sb("PT", [128, 2 * TT], BF16)
        self.ST = [self.sb(f"ST{j}", [128, NCH * 128], BF16) for j in range(2)]
        self.xprev = [self.sb(f"xprev{j}", [128, NCH], F32) for j in range(2)]
        self.PC = self.sb("PC", [128, NCH * 4], F32)
        self.PCX = self.sb("PCX", [128, NCH * 8], F32)
        self.small = self.sb("small", [128, 64], F32)
        self.TW = self.sb("TW", [128, 256], BF16)
        self.TA = self.sb("TA", [128, 256], BF16)
        self.TV = self.sb("TV", [128, 256], BF16)
        self.TG1 = self.sb("TG1", [128, 256], BF16)
        self.TG2 = self.sb("TG2", [128, 256], BF16)
        for s in ("x", "p", "y", "c0", "c1"):
            fw.new_sem("io_" + s)
        self.NCS = 8
        self.ctok = []
        for i in range(self.NCS):
            fw.new_sem(f"cast{i}")
            self.ctok.append(T())
        fw.dma("sp", "io_c0", self.P1.ap, self.P1d, writes=self.P1.ts)
        fw.dma("pool", "io_c1", self.CSTb.ap, self.CSTd, writes=self.CSTb.ts)
        for j in range(2):
            self.op("dve", "tensor_scalar", self.pc(f"omka{j}", 0, 8), self.pc(f"ka{j}", 0, 8), -1.0, 1.0,
                    ALU.mult, ALU.add)
        for j in range(2):
            self.op("dve", "memset", self.ST[j], 0.0)
            self.op("dve", "memset", self.xprev[j], 0.0)
        ci = 0
        fw.dma("pool", f"cast{ci}", self.Pb, self.pT, writes=[self.pbT, self.ctok[ci]])
        ci += 1
        for n in self.names:
            off, k = self.offs[n]
            s = ci % self.NCS
            fw.dma("pool", f"cast{s}", self.Wb[:, off:off + k], self.W[:, off:off + k],
                   writes=[self.wbT[n], self.ctok[s]])
            ci += 1
        self.ws = WStream(self, self.names, self.offs, self.ntiles)

    def rms_stats(self, src, sq, mat, epsname, n=TT):
        for k in range(NCH):
            self.op("act", "activation", sq[k], src[k], AF.Square)
        b = self.bank()
        bo = b[:, 0:n]
        self.mm(bo, [(self.cm(mat), sq[k]) for k in range(NCH)])
        r = self.rstd[self.rstd_i][:, 0:n]
        self.rstd_i ^= 1
        self.op("act", "activation", r, bo, AF.Sqrt, bias=self.pc(epsname), scale=1.0)
        self.op("dve", "reciprocal", r, r)
        return r

    def gcol(self, l, i, k):
        return self.pc("g", (l * 8 + i) * 8 + k)

    def norm_to(self, src, outs, l, i, rstd):
        for k in range(NCH):
            self.op("dve", "scalar_tensor_tensor", outs[k], src[k], self.gcol(l, i, k), rstd, ALU.mult, ALU.mult)

    def add_to_h(self, M, l, i, rstd, hsl=None):
        for k in range(NCH):
            h = self.Hc[k] if hsl is None else self.Hc[k][:, hsl[0]:hsl[1]]
            self.op("dve", "scalar_tensor_tensor", M[k], M[k], self.gcol(l, i, k), rstd, ALU.mult, ALU.mult)
            self.op("dve", "tensor_tensor", h, h, M[k], ALU.add)

    def ffn(self, l, i):
        XN = Role(self, 0, 8, BF16).chunks(8, TT)
        SQ = Role(self, 8, 8, BF16).chunks(8, TT)
        ACTT = Role(self, 16, 22, BF16).chunks(NFC, TT)
        M = Role(self, 38, 16, F32).chunks(8, TT)
        SG = Role(self, 54, 4, F32).chunks(2, TT)
        rstd = self.rms_stats(self.Hc, SQ, "ones_mean", "eps_rms")
        self.norm_to(self.Hc, XN, l, 4 * i, rstd)
        for b in range(11):
            w = self.ws.get(f"L{l}.f{i}.w13.{b}")
            wv = w.re("p (k g c) -> p k g c", k=8, g=2, c=256)
            for sub in range(2):
                fc = 2 * b + sub
                pg = self.bank()
                pu = self.bank()
                self.mm(pg, [(wv[:, k, 0, sub * 128:(sub + 1) * 128], XN[k]) for k in range(NCH)])
                self.mm(pu, [(wv[:, k, 1, sub * 128:(sub + 1) * 128], XN[k]) for k in range(NCH)])
                sg = SG[fc % 2]
                self.op("act", "activation", sg, pg, AF.Silu)
                self.op("dve", "tensor_tensor", ACTT[fc], sg, pu, ALU.mult)
        for c in range(8):
            w = self.ws.get(f"L{l}.f{i}.w2.{c}")
            wv = w[:, 0:NFC * 128].re("p (f c) -> p f c", f=NFC, c=128)
            po = self.bank()
            self.mm(po, [(wv[:, fc, :], ACTT[fc]) for fc in range(NFC)])
            self.op("act", "copy", M[c], po)
        rstd = self.rms_stats(M, SQ, "ones_q", "eps_rms4")
        self.add_to_h(M, l, 4 * i + 1, rstd)

    def ple(self, l, t):
        XN = Role(self, 0, 8, BF16).chunks(8, TT)
        SQ = Role(self, 8, 8, BF16).chunks(8, TT)
        M = Role(self, 38, 16, F32).chunks(8, TT)
        SG = Role(self, 54, 4, F32).chunks(2, TT)
        self.fw.dma("sp", "io_p", self.PT.ap.rearrange("p (k t) -> p k t", k=2),
                    self.Pb[:, l, :, t * TT:(t + 1) * TT], reads=[self.pbT], writes=self.PT.ts)
        rstd = self.rms_stats(self.Hc, SQ, "ones_mean", "eps_rms")
        self.norm_to(self.Hc, XN, l, 6, rstd)
        wp = self.ws.get(f"L{l}.p.wp", hold=True).re("p (k c) -> p k c", k=2, c=1024)
        for q in range(2):
            wg = self.ws.get(f"L{l}.p.wg.{q}").re("p (k c) -> p k c", k=8, c=512)
            for c4 in range(4):
                c = 4 * q + c4
                bg = self.bank()
                bp = self.bank()
                self.mm(bg, [(wg[:, k, c4 * 128:(c4 + 1) * 128], XN[k]) for k in range(NCH)])
                self.mm(bp, [(wp[:, k2, c * 128:(c + 1) * 128], self.PT[:, k2 * TT:(k2 + 1) * TT]) for k2 in range(2)])
                sg = SG[c % 2]
                self.op("act", "activation", sg, bg, AF.Sigmoid)
                self.op("dve", "tensor_tensor", M[c], sg, bp, ALU.mult)
        self.ws.release(f"L{l}.p.wp")
        rstd = self.rms_stats(M, SQ, "ones_mean", "eps_rms")
        self.add_to_h(M, l, 7, rstd)

    def gmlp(self, l):
        j = l // 2
        XN = Role(self, 0, 8, BF16).chunks(8, TT)
        SQ = Role(self, 8, 8, BF16).chunks(8, TT)
        UTr = Role(self, 16, 16, BF16)
        UT = UTr.chunks(16, TT)
        M = Role(self, 38, 16, F32).chunks(8, TT)
        VTOK = Role(self, 54, 32, F32).chunks(4, 2048)
        VN = Role(self, 86, 8, BF16).chunks(2, 2048)
        rstd = self.rms_stats(self.Hc, SQ, "ones_mean", "eps_rms")
        self.norm_to(self.Hc, XN, l, 2, rstd)
        for q in range(4):
            w = self.ws.get(f"L{l}.g.wu.{q}").re("p (k c) -> p k c", k=8, c=512)
            for c4 in range(4):
                ec = 4 * q + c4
                b = self.bank()
                self.mm(b, [(w[:, k, c4 * 128:(c4 + 1) * 128], XN[k]) for k in range(NCH)])
                self.op("act", "activation", UT[ec], b, AF.Gelu, bias=self.pc(f"binu{j}", ec), scale=1.0)
        rows = self.ws.get(f"L{l}.g.rows", hold=True)
        ones_row = self.cm("ones1", 0, 1, 0, 128)
        for es in range(4):
            w = self.ws.get(f"L{l}.g.wv.{es}").re("p (k c) -> p k c", k=8, c=512)
            for tb in range(4):
                b = self.bank()
                terms = [(XN[k][:, tb * 128:(tb + 1) * 128], w[:, k, :]) for k in range(NCH)]
                terms.append((ones_row, rows[0:1, es * 512:(es + 1) * 512]))
                self.mm(b, terms)
                self.op("act", "activation", VTOK[tb][:, es * 512:(es + 1) * 512], b, AF.Gelu)
        vg = self.ws.get(f"L{l}.g.vg", hold=True)
        wsb = self.ws.get(f"L{l}.g.ws", hold=True)
        wsv = wsb[:, 0:1024].re("p (g i) -> p g i", g=8, i=128)
        self.op("dve", "memset", wsv[64:128, :, 0:64], 0.0)
        sm = self.small
        for tb in range(4):
            for c in range(4):
                self.op("dve", "bn_stats", sm[:, c * 6:(c + 1) * 6], VTOK[tb][:, c * 512:(c + 1) * 512])
            self.op("dve", "bn_aggr", sm[:, 24:26], sm[:, 0:24].re("p (c d) -> p c d", d=6))
            self.op("act", "activation", sm[:, 26:27], sm[:, 25:26], AF.Sqrt, bias=self.pc("eps_ln"), scale=1.0)
            self.op("dve", "reciprocal", sm[:, 26:27], sm[:, 26:27])
            self.op("dve", "scalar_tensor_tensor", sm[:, 27:28], sm[:, 24:25], -1.0, sm[:, 26:27], ALU.mult, ALU.mult)
            self.op("act", "activation", VTOK[tb], VTOK[tb], AF.Identity, bias=sm[:, 27:28], scale=sm[:, 26:27])
            vn = VN[tb % 2]
            self.op("dve", "tensor_tensor", vn, VTOK[tb], vg[:, 0:2048], ALU.mult)
            for q in range(4):
                b = self.bank()
                for c4 in range(4):
                    ec = 4 * q + c4
                    gi = ec // 2
                    bo = b[:, c4 * 128:(c4 + 1) * 128]
                    self.mm(bo, [(vn[:, ec * 128:(ec + 1) * 128], wsv[:, gi, :]),
                                 (ones_row, rows[0:1, 2048 + gi * 128:2048 + (gi + 1) * 128])])
                uv = V(UTr.ap.rearrange("p (e t) -> p e t", e=16, t=TT)[:, 4 * q:4 * q + 4, tb * 128:(tb + 1) * 128],
                       [UT[4 * q + c4].ts[0] for c4 in range(4)])
                self.op("dve", "tensor_tensor", uv, b.re("p (c i) -> p c i", c=4, i=128), uv, ALU.mult)
        for nm in ("rows", "vg", "ws"):
            self.ws.release(f"L{l}.g.{nm}")
        for q in range(4):
            w = self.ws.get(f"L{l}.g.wo.{q}").re("p (e c) -> p e c", e=16, c=256)
            for c2 in range(2):
                c = 2 * q + c2
                b = self.bank()
                self.mm(b, [(w[:, ec, c2 * 128:(c2 + 1) * 128], UT[ec]) for ec in range(16)])
                self.op("act", "copy", M[c], b)
        rstd = self.rms_stats(M, SQ, "ones_mean", "eps_rms")
        self.add_to_h(M, l, 3, rstd)


    def rwkv(self, l, t):
        for s in range(2):
            self.rwkv_sub(l, t, s)

    def rwkv_sub(self, l, t, s):
        j = l // 2
        TR = 256
        c0 = s * TR
        R = lambda u0, nu, dt=BF16: Role(self, u0, nu, dt)
        X, DX = R(0, 4), R(4, 4)
        XM = [R(8, 4), R(12, 4)]
        rK, rKK, rA, rR, rV, SQ = R(16, 4), R(20, 4), R(24, 4), R(28, 4), R(32, 4), R(36, 4)
        F3, F4 = R(40, 8, F32), R(48, 8, F32)
        AR, BK = R(56, 8), R(64, 8)
        ET = R(72, 3, F32).chunks(3, TR)
        A_ = [R(75, 2), R(77, 2)]
        N_ = [R(79, 2), R(81, 2)]
        T_ = [R(83, 2), R(85, 2)]
        TM = [R(87, 2), R(89, 2)]
        GM = [R(91, 4), R(95, 4)]
        UV = [R(99, 2), R(101, 2)]
        BKT = [R(103, 2), R(105, 2)]
        RHST = R(107, 2)
        VT = [R(109, 2), R(111, 2)]
        SN = R(113, 2)
        Xc, DXc = X.chunks(8, TR), DX.chunks(8, TR)
        XMc = [XM[0].chunks(8, TR), XM[1].chunks(8, TR)]
        rKc, rKKc, rAc, rRc, rVc, SQc = (r.chunks(8, TR) for r in (rK, rKK, rA, rR, rV, SQ))
        F3c, F4c = F3.chunks(8, TR), F4.chunks(8, TR)
        AR3 = [AR.v(c * 512, (c + 1) * 512).re("p (tc x) -> p tc x", tc=4, x=128) for c in range(8)]
        BK3 = [BK.v(c * 512, (c + 1) * 512).re("p (tc x) -> p tc x", tc=4, x=128) for c in range(8)]
        v3 = lambda vv: vv.re("p (tc x) -> p tc x", tc=4, x=64)
        Hs = [self.Hc[k][:, c0:c0 + TR] for k in range(NCH)]
        ident = self.cm("ident")
        blk1 = self.cm("blk_1")
        blkg = self.cm("blk_gn")

        rstd = self.rms_stats(Hs, SQc, "ones_mean", "eps_rms", n=TR)
        self.norm_to(Hs, Xc, l, 2, rstd)
        X3 = X.v().re("p (k t) -> p k t", k=8, t=TR)
        DX3 = DX.v().re("p (k t) -> p k t", k=8, t=TR)
        xp3 = self.xprev[j].re("p (k o) -> p k o", k=8, o=1)
        self.op("dve", "tensor_tensor", DX3[:, :, 1:TR], X3[:, :, 0:TR - 1], X3[:, :, 1:TR], ALU.subtract)
        self.op("dve", "tensor_tensor", DX3[:, :, 0:1], xp3, X3[:, :, 0:1], ALU.subtract)
        self.op("act", "copy", xp3, X3[:, :, TR - 1:TR])

        def mix(m, dst):
            for k in range(NCH):
                self.op("dve", "scalar_tensor_tensor", dst[k], DXc[k], self.pc(f"mu{j}", m * 8 + k), Xc[k],
                        ALU.mult, ALU.add)
            return dst

        la = self.ws.get(f"L{l}.r.la.{s}", hold=True)
        lb = self.ws.get(f"L{l}.r.lb.{s}", hold=True)
        w1 = la[:, 0:512].re("p (k c) -> p k c", k=8, c=64)
        a1 = la[:, 512:1024].re("p (k c) -> p k c", k=8, c=64)
        g1 = la[:, 1024:2304].re("p (k c) -> p k c", k=8, c=160)
        v1 = la[:, 2304:2560].re("p (k c) -> p k c", k=8, c=32)
        w2, a2, v2 = lb[:, 0:1024], lb[:, 1024:2048], lb[:, 2048:3072]

        xw = mix(3, XMc[0])
        b = self.bank()
        self.mm(b[0:64, 0:TR], [(w1[:, k, :], xw[k]) for k in range(NCH)])
        self.op("act", "activation", self.TW[0:64, 0:TR], b[0:64, 0:TR], AF.Tanh)
        for c in range(8):
            if c % 2 == 0:
                b = self.bank()
            bo = b[:, (c % 2) * TR:(c % 2 + 1) * TR]
            self.mm(bo, [(w2[0:64, c * 128:(c + 1) * 128], self.TW[0:64, 0:TR])])
            self.op("act", "activation", F3c[c], bo, AF.Sigmoid, bias=self.pc(f"w0{j}", c), scale=1.0)
        src = F3.v().re("p (g t) -> p g t", t=64)
        dst = F4.v().re("p (g t) -> p g t", t=64)
        for sft in (1, 2, 4, 8, 16, 32):
            self.op("dve", "tensor_tensor", dst[:, :, sft:64], src[:, :, sft:64], src[:, :, 0:64 - sft], ALU.add)
            self.op("act", "copy", dst[:, :, 0:sft], src[:, :, 0:sft])
            src, dst = dst, src
        self.op("act", "activation", F4.v(), F3.v(), AF.Exp, scale=-C0)
        self.op("act", "activation", F3.v(), F3.v(), AF.Exp, scale=C0)
        PC3 = self.PC.re("p (k c) -> p k c", k=8, c=4)
        self.op("dve", "tensor_copy", PC3, F4.v().re("p (k c t) -> p k c t", k=8, c=4, t=64)[:, :, :, 63])

        PCX = self.PCX.re("p (k e c) -> p k e c", k=8, e=2, c=4)
        self.op("act", "copy", PCX[0:64, :, 0, :], PC3[0:64, :, :])
        self.op("dve", "tensor_copy", PCX[0:64, :, 1, :], PC3[64:128, :, :])

        xa = mix(4, XMc[1])
        b = self.bank()
        self.mm(b[0:64, 0:TR], [(a1[:, k, :], xa[k]) for k in range(NCH)])
        self.op("act", "copy", self.TA[0:64, 0:TR], b[0:64, 0:TR])
        for c in range(8):
            if c % 2 == 0:
                b = self.bank()
            bo = b[:, (c % 2) * TR:(c % 2 + 1) * TR]
            self.mm(bo, [(a2[0:64, c * 128:(c + 1) * 128], self.TA[0:64, 0:TR])])
            self.op("act", "activation", rAc[c], bo, AF.Sigmoid, bias=self.pc(f"a0{j}", c), scale=1.0)

        def proj(n, xm, outc):
            for q in range(2):
                w = self.ws.get(f"L{l}.r.win{n}.{q}.{s}").re("p (k c) -> p k c", k=8, c=512)
                for c4 in range(4):
                    c = 4 * q + c4
                    bb = self.bank()
                    self.mm(bb[:, 0:TR], [(w[:, k, c4 * 128:(c4 + 1) * 128], xm[k]) for k in range(NCH)])
                    self.op("act", "copy", outc[c], bb[:, 0:TR])

        xk = mix(1, XMc[0])
        proj(1, xk, rKc)
        for c in range(8):
            e0, e1 = ET[0], ET[1]
            self.op("dve", "tensor_scalar_mul", rKKc[c], rKc[c], self.pc(f"kk{j}", c))
            self.op("act", "activation", SQc[c], rKKc[c], AF.Square)
            bb = self.bank()
            self.mm(bb[:, 0:TR], [(blk1, SQc[c])])
            self.op("dve", "tensor_scalar_max", e0, bb[:, 0:TR], 1e-24)
            self.op("act", "activation", e0, e0, AF.Sqrt)
            self.op("dve", "reciprocal", e0, e0)
            self.op("dve", "tensor_tensor", rKKc[c], rKKc[c], e0, ALU.mult)
            self.op("dve", "tensor_scalar", e1, rAc[c], self.pc(f"ka{j}", c), self.pc(f"omka{j}", c),
                    ALU.mult, ALU.add)
            self.op("dve", "tensor_tensor", rKc[c], rKc[c], e1, ALU.mult)
            self.op("dve", "tensor_tensor", e1, rKKc[c], rAc[c], ALU.mult)
            self.op("dve", "tensor_tensor", BK3[c][:, :, 0:64], v3(e1), v3(F3c[c]), ALU.mult)
            self.op("dve", "tensor_tensor", BK3[c][:, :, 64:128], v3(rKc[c]), v3(F3c[c]), ALU.mult)
            self.op("dve", "scalar_tensor_tensor", AR3[c][:, :, 1:64], v3(rKKc[c])[:, :, 1:64], -1.0,
                    v3(F4c[c])[:, :, 0:63], ALU.mult, ALU.mult)
            self.op("act", "mul", AR3[c][:, :, 0:1], v3(rKKc[c])[:, :, 0:1], -1.0)

        xr = mix(0, XMc[1])
        proj(0, xr, rRc)
        for c in range(8):
            self.op("dve", "tensor_tensor", AR3[c][:, :, 64:128], v3(rRc[c]), v3(F4c[c]), ALU.mult)
            self.op("dve", "scalar_tensor_tensor", SQc[c], rRc[c], self.pc(f"rk{j}", c), rKc[c], ALU.mult, ALU.mult)

        xv = mix(2, XMc[0])
        proj(2, xv, rVc)
        if j >= 1:
            b = self.bank()
            self.mm(b[0:32, 0:TR], [(v1[:, k, :], xv[k]) for k in range(NCH)])
            self.op("act", "copy", self.TV[0:32, 0:TR], b[0:32, 0:TR])
        for c in range(8):
            e0, e1 = ET[0], ET[1]
            if j >= 1:
                bb = self.bank()
                self.mm(bb[:, 0:TR], [(v2[0:32, c * 128:(c + 1) * 128], self.TV[0:32, 0:TR])])
                self.op("act", "activation", e0, bb[:, 0:TR], AF.Sigmoid, bias=self.pc(f"v0{j}", c), scale=1.0)
                self.op("dve", "tensor_tensor", e1, self.VFc[c][:, c0:c0 + TR], rVc[c], ALU.subtract)
                self.op("dve", "tensor_tensor", e1, e1, e0, ALU.mult)
                self.op("dve", "tensor_tensor", rVc[c], rVc[c], e1, ALU.add)
            else:
                self.op("act", "copy", self.VFc[c][:, c0:c0 + TR], rVc[c])
            bb = self.bank()
            self.mm(bb[:, 0:TR], [(blk1, SQc[c])])
            self.op("dve", "tensor_tensor", rAc[c], bb[:, 0:TR], rVc[c], ALU.mult)

        xg = mix(5, XMc[1])
        lc = self.ws.get(f"L{l}.r.lc.{s}", hold=True)
        g2a, g2b = lc[:, 0:1024], lc[:, 1024:2048]
        b1 = self.bank()
        self.mm(b1[:, 0:TR], [(g1[:, k, 0:128], xg[k]) for k in range(NCH)])
        b2 = self.bank()
        self.mm(b2[0:32, 0:TR], [(g1[:, k, 128:160], xg[k]) for k in range(NCH)])
        self.op("act", "activation", self.TG1[:, 0:TR], b1[:, 0:TR], AF.Sigmoid)
        self.op("act", "activation", self.TG2[0:32, 0:TR], b2[0:32, 0:TR], AF.Sigmoid)
        for c in range(8):
            bb = self.bank()
            self.mm(bb[:, 0:TR], [(g2a[:, c * 128:(c + 1) * 128], self.TG1[:, 0:TR]),
                                  (g2b[0:32, c * 128:(c + 1) * 128], self.TG2[0:32, 0:TR])])
            self.op("act", "copy", rKKc[c], bb[:, 0:TR])
        for nm in ("la", "lb", "lc"):
            self.ws.release(f"L{l}.r.{nm}.{s}")

        ST4 = self.ST[j].re("p (k h i) -> p k h i", k=8, h=2, i=64)
        Y4 = F4.v().re("p (k c t) -> p k c t", k=8, c=4, t=64)
        maskg = self.cm("maskg4", 0, 128, 0, 512)
        maskn = self.cm("maskn16", 0, 64, 0, 1024)
        id16 = self.cm("ident16", 0, 64, 0, 1024)
        for st in range(2):
            self.op("dve", "memset", VT[st].v()[0:64, :], 0.0)
        for tc in range(4):
            st = tc % 2
            gm3 = GM[st].v().re("p (h x) -> p h x", h=16, x=128)
            uv3 = UV[st].v().re("p (h i) -> p h i", h=16, i=64)
            bkt3 = BKT[st].v().re("p (h i) -> p h i", h=16, i=64)
            vt3 = VT[st].v().re("p (h i) -> p h i", h=16, i=64)
            for hf in range(2):
                bt = self.bank()
                for d4 in range(4):
                    dc = 4 * hf + d4
                    self.op("pe", "matmul", bt[:, d4 * 128:(d4 + 1) * 128], BK3[dc][:, tc, :], ident,
                            start=True, stop=True, inc=(d4 == 3))
                self.op("act", "copy", BKT[st].v()[:, hf * 512:(hf + 1) * 512], bt)
            for hf in range(2):
                bv = self.bank()
                for d4 in range(4):
                    dc = 4 * hf + d4
                    self.op("pe", "matmul", bv[0:64, d4 * 128:(d4 + 1) * 128], rVc[dc][:, tc * 64:(tc + 1) * 64], ident,
                            start=True, stop=True, inc=(d4 == 3))
                self.op("dve", "tensor_copy", VT[st].v()[64:128, hf * 512:(hf + 1) * 512], bv[0:64, :])
            gm4 = GM[st].v().re("p (d e x) -> p d e x", d=8, e=2, x=128)
            for hp in range(2):
                ps = slice(64 * hp, 64 * hp + 64)
                for q in range(2):
                    bg = self.bank()
                    for d4 in range(4):
                        dc = 4 * q + d4
                        self.op("pe", "matmul", bg[:, d4 * 128:(d4 + 1) * 128], BK3[dc][ps, tc, :],
                                AR3[dc][ps, tc, :], start=True, stop=True, inc=(d4 == 3))
                    self.op("dve", "tensor_tensor", gm4[:, 4 * q:4 * q + 4, hp, :],
                            bg.re("p (d x) -> p d x", d=4, x=128),
                            maskg.re("p (d x) -> p d x", d=4, x=128), ALU.mult)
            n4 = N_[0].v()[0:64, :].re("p (d e x) -> p d e x", d=8, e=2, x=64)
            for hp in range(2):
                ps = slice(64 * hp, 64 * hp + 64)
                bn1 = self.bank()
                for dc in range(8):
                    self.op("pe", "matmul", bn1[0:64, dc * 64:(dc + 1) * 64],
                            AR3[dc][ps, tc, 0:64], BK3[dc][ps, tc, 0:64], start=True, stop=True, inc=(dc == 7))
                self.op("dve", "tensor_tensor", n4[:, :, hp, :], bn1[0:64, :].re("p (d x) -> p d x", d=8, x=64),
                        maskn[:, 0:512].re("p (d x) -> p d x", d=8, x=64), ALU.mult)
            self.op("act", "copy", A_[0].v()[0:64, :].re("p (h x) -> p h x", h=16, x=64), gm3[0:64, :, 0:64])
            self.op("dve", "tensor_tensor", T_[0].v()[0:64, :], A_[0].v()[0:64, :], id16, ALU.add)
            cur = 0
            for step in range(1, 6):
                nxt = 1 - cur
                Ac, Nc, Tc = A_[cur].v()[0:64, :], N_[cur].v()[0:64, :], T_[cur].v()[0:64, :]
                An, Nn, Tn = A_[nxt].v()[0:64, :], N_[nxt].v()[0:64, :], T_[nxt].v()[0:64, :]
                if step < 5:
                    ba = [self.bank(), self.bank()]
                    for h in range(16):
                        hs = slice(h * 64, (h + 1) * 64)
                        self.op("pe", "matmul", ba[h // 8][0:64, (h % 8) * 64:(h % 8 + 1) * 64], Nc[:, hs], Ac[:, hs],
                                start=True, stop=True, inc=(h % 8 == 7))
                bn = [self.bank(), self.bank()]
                for h in range(16):
                    hs = slice(h * 64, (h + 1) * 64)
                    self.op("pe", "matmul", bn[h // 8][0:64, (h % 8) * 64:(h % 8 + 1) * 64], Ac[:, hs], Nc[:, hs],
                            start=True, stop=True, inc=(h % 8 == 7))
                for hf in range(2):
                    fs = slice(hf * 512, (hf + 1) * 512)
                    if step < 5:
                        self.op("act", "copy", An[:, fs], ba[hf][0:64, :])
                    self.op("dve", "tensor_copy", Nn[:, fs], bn[hf][0:64, :])
                bt2 = [self.bank(), self.bank()]
                for h in range(16):
                    hs = slice(h * 64, (h + 1) * 64)
                    self.op("pe", "matmul", bt2[h // 8][0:64, (h % 8) * 64:(h % 8 + 1) * 64], Nn[:, hs], Tc[:, hs],
                            start=True, stop=True, inc=(h % 8 == 7))
                for hf in range(2):
                    fs = slice(hf * 512, (hf + 1) * 512)
                    self.op("dve", "tensor_tensor", Tn[:, fs], bt2[hf][0:64, :], Tc[:, fs], ALU.add)
                cur = nxt
            Tf = T_[cur].v()[0:64, :]
            br = [self.bank(), self.bank()]
            for h in range(16):
                dc, hp = h // 2, h % 2
                out = br[h // 8][0:64, (h % 8) * 64:(h % 8 + 1) * 64]
                self.op("pe", "matmul", out, AR3[dc][:, tc, 0:64], ST4[:, dc, hp, :], start=True, stop=False, inc=False)
                self.op("pe", "matmul", out, gm3[:, h, 0:64], vt3[:, h, :], start=False, stop=True,
                        inc=(h % 8 == 7))
            self.op("act", "copy", RHST.v()[0:64, 0:512], br[0][0:64, :])
            self.op("dve", "tensor_copy", RHST.v()[0:64, 512:1024], br[1][0:64, :])
            bu = [self.bank(), self.bank()]
            for h in range(16):
                hs = slice(h * 64, (h + 1) * 64)
                self.op("pe", "matmul", bu[h // 8][0:64, (h % 8) * 64:(h % 8 + 1) * 64], Tf[:, hs],
                        RHST.v()[0:64, hs], start=True, stop=True, inc=(h % 8 == 7))
            self.op("act", "copy", UV[st].v()[0:64, 0:512], bu[0][0:64, :])
            self.op("dve", "tensor_copy", UV[st].v()[0:64, 512:1024], bu[1][0:64, :])
            self.op("dve", "tensor_copy", UV[st].v()[64:128, :], VT[st].v()[64:128, :])
            by = [self.bank(), self.bank()]
            for h in range(16):
                dc, hp = h // 2, h % 2
                out = by[h // 8][0:64, (h % 8) * 64:(h % 8 + 1) * 64]
                self.op("pe", "matmul", out, ST4[:, dc, hp, :], AR3[dc][:, tc, 64:128], start=True, stop=False,
                        inc=False)
                self.op("pe", "matmul", out, uv3[:, h, :], gm3[:, h, 64:128], start=False, stop=True,
                        inc=(h % 8 == 7))
            bs = [self.bank(), self.bank()]
            for h in range(16):
                dc, hp = h // 2, h % 2
                out = bs[h // 8][0:64, (h % 8) * 64:(h % 8 + 1) * 64]
                self.op("pe", "matmul", out, bkt3[:, h, :], uv3[:, h, :], start=True, stop=False, inc=False)
                self.op("pe", "matmul", out, ident[:, 64 * hp:64 * hp + 64], ST4[:, dc, hp, :], start=False, stop=True,
                        inc=(h % 8 == 7))
            for hf in range(2):
                byv = by[hf][0:64, :].re("p (d e t) -> p d e t", d=4, e=2, t=64)
                for hp in range(2):
                    ps = slice(64 * hp, 64 * hp + 64)
                    eng = "act" if hp == 0 else "dve"
                    self.op(eng, "copy" if hp == 0 else "tensor_copy", Y4[ps, 4 * hf:4 * hf + 4, tc, :], byv[:, :, hp, :])
            sn = SN.v()[0:64, :]
            for hf in range(2):
                self.op("dve", "tensor_tensor", sn[:, hf * 512:(hf + 1) * 512].re("p (d e i) -> p d e i", d=4, e=2, i=64),
                        bs[hf][0:64, :].re("p (d e i) -> p d e i", d=4, e=2, i=64),
                        PCX[0:64, 4 * hf:4 * hf + 4, :, tc:tc + 1].bc([64, 4, 2, 64]), ALU.mult)
            sn4 = sn.re("p (d e i) -> p d e i", d=8, e=2, i=64)
            self.op("act", "copy", ST4[0:64, :, 0, :], sn4[:, :, 0, :])
            self.op("dve", "tensor_copy", ST4[64:128, :, 1, :], sn4[:, :, 1, :])

        xo = XMc[0]
        for c in range(8):
            e0 = ET[0]
            y = F4c[c]
            self.op("act", "copy", SQc[c], y)
            bb = self.bank()
            self.mm(bb[:, 0:TR], [(blkg, SQc[c])])
            self.op("dve", "tensor_tensor", y, y, bb[:, 0:TR], ALU.subtract)
            self.op("act", "activation", SQc[c], y, AF.Square)
            bb = self.bank()
            self.mm(bb[:, 0:TR], [(blkg, SQc[c])])
            self.op("act", "activation", e0, bb[:, 0:TR], AF.Sqrt, bias=self.pc("eps_gn"), scale=1.0)
            self.op("dve", "reciprocal", e0, e0)
            self.op("dve", "tensor_tensor", y, y, e0, ALU.mult)
            self.op("dve", "tensor_scalar", y, y, self.pc(f"ln0{j}", c), self.pc(f"ln1{j}", c), ALU.mult, ALU.add)
            self.op("dve", "tensor_tensor", y, y, rAc[c], ALU.add)
            self.op("dve", "tensor_tensor", xo[c], y, rKKc[c], ALU.mult)
        Mc = F3c
        for q in range(2):
            w = self.ws.get(f"L{l}.r.wo.{q}.{s}").re("p (k c) -> p k c", k=8, c=512)
            for c4 in range(4):
                c = 4 * q + c4
                bb = self.bank()
                self.mm(bb[:, 0:TR], [(w[:, k, c4 * 128:(c4 + 1) * 128], xo[k]) for k in range(NCH)])
                self.op("act", "copy", Mc[c], bb[:, 0:TR])
        rstd = self.rms_stats(Mc, SQc, "ones_mean", "eps_rms", n=TR)
        self.add_to_h(Mc, l, 3, rstd, hsl=(c0, c0 + TR))

    def load_x(self, t):
        self.fw.dma("sp", "io_x", self.H.ap.rearrange("p (k t) -> p k t", k=NCH),
                    self.xT[:, :, t * TT:(t + 1) * TT], writes=self.H.ts)

    def store(self, t):
        self.fw.dma("sp", "io_y", self.yT[:, :, t * TT:(t + 1) * TT],
                    self.H.ap.rearrange("p (k t) -> p k t", k=NCH), reads=self.H.ts)

    def run(self, stages=None):
        self.setup()
        for t in range(self.ntiles):
            self.load_x(t)
            for l in range(self.nlayers):
                self.ffn(l, 0)
                if l % 2 == 0:
                    self.gmlp(l)
                else:
                    self.rwkv(l, t)
                self.ffn(l, 1)
                self.ple(l, t)
            self.store(t)
        self.fw.wait_all("sp", self.H.ts)
        self.fw.emit()


def build_nc(blocks_meta, ntiles, nlayers, s_core):
    nc = bass.Bass("TRN2", target_bir_lowering=False)
    with ExitStack() as st:
        g = Gen(nc, st, blocks_meta, ntiles, nlayers, s_core)
        g.run()
        n_ins = g.fw.n_ins
    return nc, n_ins


def prep_shared(inp, nlayers=4):
    blocks = block_list(inp, nlayers)
    meta = [(n, a.shape[1]) for n, a in blocks]
    W = np.concatenate([a for _, a in blocks], axis=1)
    return meta, W, build_p1(inp), build_cst()


def prep_core(x_b, p_b):
    S = x_b.shape[0]
    xT = np.ascontiguousarray(x_b.reshape(S, NCH, 128).transpose(2, 1, 0))
    pT = np.ascontiguousarray(p_b.reshape(4, S, 2, 128).transpose(3, 0, 2, 1))
    return xT, pT


def kernel(**inputs):
    inp = {k: np.asarray(v, np.float32) for k, v in inputs.items()}
    meta, W, P1, CSTa = prep_shared(inp)
    B = inp["x"].shape[0]
    nc, _ = build_nc(meta, SEQ // TT, 4, SEQ)
    in_maps = []
    for b in range(B):
        xT, pT = prep_core(inp["x"][b], inp["p"][:, b])
        in_maps.append({"xT": xT, "pT": pT, "W": W, "P1": P1, "CST": CSTa})
    res = run_bass_kernel_spmd(nc, in_maps, core_ids=list(range(B)))
    out = np.empty((B, SEQ, D), np.float32)
    for b in range(B):
        yT = res.results[b]["yT"]
        out[b] = yT.transpose(2, 1, 0).reshape(SEQ, D)
    return out
```

```python
import math
from contextlib import ExitStack

import numpy as np
import concourse.bass as bass
import concourse.mybir as mybir
from concourse.bass_utils import run_bass_kernel_spmd

F32 = mybir.dt.float32
BF16 = mybir.dt.bfloat16
AF = mybir.ActivationFunctionType
ALU = mybir.AluOpType
AX = mybir.AxisListType

D = 1024
NCH = 8
TT = 512
SEQ = 4096
DFF = 2816
NFC = 22
SLOT = 4096
RING = 5
C0 = math.exp(-0.5)


class T:
    __slots__ = ("w", "r")

    def __init__(self):
        self.w = None
        self.r = {}


class V:
    __slots__ = ("ap", "ts")

    def __init__(self, ap, ts):
        self.ap = ap
        self.ts = ts

    def __getitem__(self, idx):
        return V(self.ap[idx], self.ts)

    def re(self, pat, **kw):
        return V(self.ap.rearrange(pat, **kw), self.ts)

    def bc(self, shape):
        return V(self.ap.broadcast_to(list(shape)), self.ts)


class Eng:
    def __init__(self, name, same_wait):
        self.name = name
        self.items = []
        self.count = 0
        self.seen = {}
        self.same_wait = same_wait
        self.semkey = "E_" + name


class FW:
    def __init__(self, nc, stack):
        self.nc = nc
        self.stack = stack
        self.sems = {}
        self.engs = {}
        for name, sw in (("pe", False), ("act", True), ("dve", True), ("pool", True), ("sp", False)):
            e = Eng(name, sw)
            self.engs[name] = e
            self.sems[e.semkey] = stack.enter_context(nc.semaphore("s_" + name))
        self.dma_cnt = {}
        self.n_ins = 0

    def new_sem(self, key):
        self.sems[key] = self.stack.enter_context(self.nc.semaphore(key))
        self.dma_cnt[key] = 0
        return key

    def _waits(self, eng, reads, writes):
        need = {}

        def add(ev):
            if ev is None:
                return
            k, v = ev
            if k == eng.semkey and not eng.same_wait:
                return
            if eng.seen.get(k, 0) >= v:
                return
            if need.get(k, 0) < v:
                need[k] = v

        for t in reads:
            add(t.w)
        for t in writes:
            add(t.w)
            for k, v in t.r.items():
                add((k, v))
        for k, v in need.items():
            eng.seen[k] = v
            h = self.sems[k]
            if k == eng.semkey:
                assert v <= eng.count, "self-wait on pending event"
            eng.items.append(("w", h, v))

    def op(self, engname, meth, args, kw, reads, writes, inc=True):
        eng = self.engs[engname]
        self._waits(eng, reads, writes)
        self.n_ins += 1
        if inc:
            eng.count += 1
            eng.items.append(("i", meth, args, kw, self.sems[eng.semkey], 1))
            ev = (eng.semkey, eng.count)
        else:
            eng.items.append(("i", meth, args, kw, None, 0))
            ev = (eng.semkey, eng.count + 1)
        for t in reads:
            if t.r.get(ev[0], 0) < ev[1]:
                t.r[ev[0]] = ev[1]
        for t in writes:
            t.w = ev
            t.r = {}
        return ev

    def dma(self, engname, semkey, out, in_, reads=(), writes=()):
        eng = self.engs[engname]
        self._waits(eng, reads, writes)
        self.n_ins += 1
        self.dma_cnt[semkey] += 16
        eng.items.append(("i", "dma_start", (), {"out": out, "in_": in_}, self.sems[semkey], 16))
        ev = (semkey, self.dma_cnt[semkey])
        for t in reads:
            if t.r.get(ev[0], 0) < ev[1]:
                t.r[ev[0]] = ev[1]
        for t in writes:
            t.w = ev
            t.r = {}
        return ev

    def wait_all(self, engname, tiles):
        self._waits(self.engs[engname], (), tiles)

    def emit(self):
        def run(e, items):
            for it in items:
                if it[0] == "w":
                    e.wait_ge(it[1], it[2])
                else:
                    _, meth, args, kw, sem, n = it
                    ins = getattr(e, meth)(*args, **kw)
                    if sem is not None:
                        ins.then_inc(sem, n)

        with self.nc.Block() as block:
            @block.tensor
            def _(e):
                run(e, self.engs["pe"].items)

            @block.scalar
            def _(e):
                run(e, self.engs["act"].items)

            @block.vector
            def _(e):
                run(e, self.engs["dve"].items)

            @block.gpsimd
            def _(e):
                run(e, self.engs["pool"].items)

            @block.sync
            def _(e):
                run(e, self.engs["sp"].items)


def _kmaj(w):
    K, N = w.shape
    return w.reshape(K // 128, 128, N).transpose(1, 0, 2)


def _fm(v):
    return np.asarray(v, np.float32).reshape(-1, 128).T


def _pad_rows(a):
    out = np.zeros((128,) + a.shape[1:], np.float32)
    out[: a.shape[0]] = a
    return out


def block_list(inp, nlayers=4):
    blocks = []

    def add(name, arr):
        a = np.ascontiguousarray(np.asarray(arr, np.float32).reshape(128, -1))
        assert a.shape[1] <= SLOT, (name, a.shape)
        blocks.append((name, a))

    def ffn(l, i):
        w13k = _kmaj(inp["ffn_w13"][l, i])
        for b in range(11):
            g = w13k[:, :, 256 * b:256 * b + 256]
            u = w13k[:, :, DFF + 256 * b:DFF + 256 * b + 256]
            add(f"L{l}.f{i}.w13.{b}", np.stack([g, u], axis=2))
        w2k = _kmaj(inp["ffn_w2"][l, i])
        for c in range(8):
            add(f"L{l}.f{i}.w2.{c}", w2k[:, :, 128 * c:128 * c + 128])

    for l in range(nlayers):
        ffn(l, 0)
        j = l // 2
        if l % 2 == 0:
            win = _kmaj(inp["a_w_in"][j])
            for q in range(4):
                add(f"L{l}.g.wu.{q}", win[:, :, 512 * q:512 * q + 512])
            rows = np.zeros((128, 3072), np.float32)
            rows[0, :2048] = inp["a_b_in"][j][2048:]
            rows[0, 2048:] = inp["a_b_s"][j].reshape(-1)
            add(f"L{l}.g.rows", rows)
            for q in range(4):
                add(f"L{l}.g.wv.{q}", win[:, :, 2048 + 512 * q:2048 + 512 * q + 512])
            add(f"L{l}.g.vg", np.broadcast_to(inp["a_v_gain"][j][None, :], (128, 2048)))
            add(f"L{l}.g.ws", inp["a_w_s"][j].transpose(2, 0, 1))
            wo = _kmaj(inp["a_w_out"][j])
            for q in range(4):
                add(f"L{l}.g.wo.{q}", wo[:, :, 256 * q:256 * q + 256])
        else:
            la = np.concatenate([
                _kmaj(inp["b_w1"][j]).reshape(128, -1), _kmaj(inp["b_a1"][j]).reshape(128, -1),
                _kmaj(inp["b_g1"][j]).reshape(128, -1),
                (_kmaj(inp["b_v1"][j - 1]).reshape(128, -1) if j >= 1 else np.zeros((128, 256), np.float32))],
                axis=1)
            lb = np.concatenate([
                _pad_rows(inp["b_w2"][j]), _pad_rows(inp["b_a2"][j]),
                (_pad_rows(inp["b_v2"][j - 1]) if j >= 1 else np.zeros((128, 1024), np.float32))], axis=1)
            lc = np.concatenate([inp["b_g2"][j][:128], _pad_rows(inp["b_g2"][j][128:])], axis=1)
            wks = [_kmaj(inp["b_w_in"][j, n]) for n in range(3)]
            wo = _kmaj(inp["b_w_out"][j])
            for s in range(2):
                add(f"L{l}.r.la.{s}", la)
                add(f"L{l}.r.lb.{s}", lb)
                for n in (1, 0, 2):
                    for q in range(2):
                        add(f"L{l}.r.win{n}.{q}.{s}", wks[n][:, :, 512 * q:512 * q + 512])
                add(f"L{l}.r.lc.{s}", lc)
                for q in range(2):
                    add(f"L{l}.r.wo.{q}.{s}", wo[:, :, 512 * q:512 * q + 512])
        ffn(l, 1)
        add(f"L{l}.p.wp", _kmaj(inp["ple_w_proj"][l]))
        wg = _kmaj(inp["ple_w_gate"][l])
        for q in range(2):
            add(f"L{l}.p.wg.{q}", wg[:, :, 512 * q:512 * q + 512])
    return blocks


def p1_layout():
    cols = {}
    n = 0

    def add(name, k):
        nonlocal n
        cols[name] = n
        n += k

    add("g", 4 * 8 * 8)
    for j in range(2):
        add(f"binu{j}", 16)
    for j in range(2):
        add(f"mu{j}", 48)
        for nm in ("w0", "a0", "kk", "ka", "rk", "ln0", "ln1", "v0", "omka"):
            add(f"{nm}{j}", 8)
    for nm in ("eps_rms", "eps_rms4", "eps_ln", "eps_gn", "tiny"):
        add(nm, 1)
    return cols, n


def build_p1(inp):
    cols, n = p1_layout()
    p = np.zeros((128, n), np.float32)
    for l in range(4):
        for i in range(8):
            c = cols["g"] + (l * 8 + i) * 8
            p[:, c:c + 8] = _fm(inp["norm_g"][l, i])
    for j in range(2):
        p[:, cols[f"binu{j}"]:cols[f"binu{j}"] + 16] = _fm(inp["a_b_in"][j][:2048])
        for m in range(6):
            c = cols[f"mu{j}"] + m * 8
            p[:, c:c + 8] = _fm(inp["b_mu"][j, m])
        for nm, key in (("w0", "b_w0"), ("a0", "b_a0"), ("kk", "b_k_k"), ("ka", "b_k_a")):
            p[:, cols[f"{nm}{j}"]:cols[f"{nm}{j}"] + 8] = _fm(inp[key][j])
        p[:, cols[f"rk{j}"]:cols[f"rk{j}"] + 8] = _fm(inp["b_r_k"][j].reshape(-1))
        p[:, cols[f"ln0{j}"]:cols[f"ln0{j}"] + 8] = _fm(inp["b_lnx"][j, 0])
        p[:, cols[f"ln1{j}"]:cols[f"ln1{j}"] + 8] = _fm(inp["b_lnx"][j, 1])
        if j >= 1:
            p[:, cols[f"v0{j}"]:cols[f"v0{j}"] + 8] = _fm(inp["b_v0"][j - 1])
    p[:, cols["eps_rms"]] = 1e-6
    p[:, cols["eps_rms4"]] = 4e-6
    p[:, cols["eps_ln"]] = 1e-5
    p[:, cols["eps_gn"]] = 64e-5
    p[:, cols["tiny"]] = 1e-24
    return p


CST = {}


def cst_layout():
    cols = {}
    n = 0
    for name, k in (("ones_mean", 128), ("ones_q", 128), ("blk_gn", 128), ("blk_1", 128), ("ident", 128),
                    ("ones1", 128), ("maskg4", 512), ("maskn16", 1024), ("ident16", 1024)):
        cols[name] = n
        n += k
    return cols, n


def build_cst():
    cols, n = cst_layout()
    c = np.zeros((128, n), np.float32)
    c[:, cols["ones_mean"]:cols["ones_mean"] + 128] = 1.0 / 1024
    c[:, cols["ones_q"]:cols["ones_q"] + 128] = 1.0 / 256
    blk = np.zeros((128, 128), np.float32)
    blk[:64, :64] = 1
    blk[64:, 64:] = 1
    c[:, cols["blk_gn"]:cols["blk_gn"] + 128] = blk / 64
    c[:, cols["blk_1"]:cols["blk_1"] + 128] = blk
    c[:, cols["ident"]:cols["ident"] + 128] = np.eye(128)
    c[:, cols["ones1"]:cols["ones1"] + 128] = 1.0
    s = np.arange(64)
    strict = (s[:, None] < s[None, :]).astype(np.float32)
    incl = (s[:, None] <= s[None, :]).astype(np.float32)
    mg = np.concatenate([np.concatenate([strict, incl], 1)] * 2, 0)
    c[:, cols["maskg4"]:cols["maskg4"] + 512] = np.tile(mg, (1, 4))
    mn = (s[:, None] > s[None, :]).astype(np.float32)
    c[:64, cols["maskn16"]:cols["maskn16"] + 1024] = np.tile(mn, (1, 16))
    c[:64, cols["ident16"]:cols["ident16"] + 1024] = np.tile(np.eye(64, dtype=np.float32), (1, 16))
    return c


class Role:
    def __init__(self, g, u0, nu, dt):
        self.g = g
        self.u0 = u0
        self.nu = nu
        base = g.arena[:, u0 * 512:(u0 + nu) * 512]
        self.ap = base if dt == BF16 else base.bitcast(F32)
        self.epu = 512 if dt == BF16 else 256
        self.n = nu * self.epu

    def v(self, c0=0, c1=None):
        c1 = self.n if c1 is None else c1
        us = range(self.u0 + c0 // self.epu, self.u0 + (c1 - 1) // self.epu + 1)
        return V(self.ap[:, c0:c1], [self.g.units[u] for u in us])

    def chunks(self, n, w):
        return [self.v(i * w, (i + 1) * w) for i in range(n)]


class WStream:
    def __init__(self, g, names, offs, ntiles):
        self.g = g
        self.names = names
        self.offs = offs
        self.total = len(names) * ntiles
        self.issued = 0
        self.pos = 0
        self.slots = []
        for i in range(RING):
            t = g.stack.enter_context(g.nc.sbuf_tensor(f"ring{i}", [128, SLOT], BF16))
            self.slots.append(V(t[:], [T()]))
            g.fw.new_sem(f"ring{i}")
        self.free = list(range(RING))
        self.loaded = {}
        self.held = {}
        self.last = None

    def pump(self):
        while self.free and self.issued < self.total:
            s = self.free.pop(0)
            name = self.names[self.issued % len(self.names)]
            off, n = self.offs[name]
            g = self.g
            g.ensure_cast(self.issued + 1 + 16)
            g.fw.dma("sp", f"ring{s}", self.slots[s].ap[:, 0:n], g.Wb[:, off:off + n],
                     reads=[g.wbT[name]], writes=self.slots[s].ts)
            self.loaded[self.issued] = s
            self.issued += 1

    def get(self, name, hold=False):
        if self.last is not None:
            self.free.append(self.last)
            self.last = None
        self.pump()
        want = self.names[self.pos % len(self.names)]
        assert want == name, (want, name)
        s = self.loaded.pop(self.pos)
        self.pos += 1
        if hold:
            self.held[name] = s
        else:
            self.last = s
        return self.slots[s][:, 0:self.offs[name][1]]

    def release(self, name):
        self.free.append(self.held.pop(name))
        self.pump()


class Gen:
    def __init__(self, nc, stack, blocks_meta, ntiles, nlayers, s_core, debug_out=None):
        self.nc = nc
        self.stack = stack
        self.fw = FW(nc, stack)
        self.ntiles = ntiles
        self.nlayers = nlayers
        self.s_core = s_core
        names = [n for n, _ in blocks_meta]
        self.offs = {}
        off = 0
        for n, k in blocks_meta:
            self.offs[n] = (off, k)
            off += k
        self.ncols = off
        self.names = names
        self.p1c, self.np1 = p1_layout()
        self.cc, self.ncst = cst_layout()
        self.xT = nc.dram_tensor("xT", [128, NCH, s_core], F32, kind="ExternalInput").ap()
        self.pT = nc.dram_tensor("pT", [128, 4, 2, s_core], F32, kind="ExternalInput").ap()
        self.W = nc.dram_tensor("W", [128, self.ncols], F32, kind="ExternalInput").ap()
        self.P1d = nc.dram_tensor("P1", [128, self.np1], F32, kind="ExternalInput").ap()
        self.CSTd = nc.dram_tensor("CST", [128, self.ncst], F32, kind="ExternalInput").ap()
        self.yT = nc.dram_tensor("yT", [128, NCH, s_core], F32, kind="ExternalOutput").ap()
        self.Wb = nc.dram_tensor("Wb", [128, self.ncols], BF16, kind="Internal").ap()
        self.Pb = nc.dram_tensor("Pb", [128, 4, 2, s_core], BF16, kind="Internal").ap()
        self.wbT = {n: T() for n in names}
        self.pbT = T()

    def sb(self, name, shape, dt=F32):
        t = self.stack.enter_context(self.nc.sbuf_tensor(name, list(shape), dt))
        return V(t[:], [T()])

    def op(self, eng, meth, out, *args, inc=True, **kw):
        reads, writes = [], list(out.ts)
        a2 = [out.ap]
        for a in args:
            if isinstance(a, V):
                reads += a.ts
                a2.append(a.ap)
            else:
                a2.append(a)
        k2 = {}
        for k, v in kw.items():
            if isinstance(v, V):
                if k == "accum_out":
                    writes += v.ts
                else:
                    reads += v.ts
                k2[k] = v.ap
            else:
                k2[k] = v
        return self.fw.op(eng, meth, tuple(a2), k2, reads, writes, inc=inc)

    def mm(self, out, terms, tps=None):
        n = len(terms)
        for i, (l, r) in enumerate(terms):
            kw = {}
            if tps is not None and tps[i] is not None:
                kw["tile_position"] = tps[i]
            self.op("pe", "matmul", out, l, r, start=(i == 0), stop=(i == n - 1), inc=(i == n - 1), **kw)

    def bank(self):
        b = self.banks[self.bank_i]
        self.bank_i = (self.bank_i + 1) % 8
        return b

    def pc(self, name, k=0, n=1):
        c = self.p1c[name] + k
        return self.P1[:, c:c + n]

    def cm(self, name, r0=0, r1=128, c0=0, c1=128):
        c = self.cc[name]
        return self.CSTb[r0:r1, c + c0:c + c1]

    def setup(self):
        nc, st, fw = self.nc, self.stack, self.fw
        self.NU = 116
        at = st.enter_context(nc.sbuf_tensor("arena", [128, self.NU * 512], BF16))
        self.arena = at[:]
        self.units = [T() for _ in range(self.NU)]
        self.banks = []
        pt = st.enter_context(nc.psum_tensor("psum_all", [128, 8 * 512], F32))
        self.psum = pt[:]
        for i in range(8):
            self.banks.append(V(self.psum[:, i * 512:(i + 1) * 512], [T()]))
        self.bank_i = 0
        self.P1 = self.sb("P1s", [128, self.np1], F32)
        self.CSTb = self.sb("CSTs", [128, self.ncst], BF16)
        self.H = self.sb("H", [128, NCH * TT], F32)
        self.Hc = []
        ht = self.H.ap
        for k in range(NCH):
            self.Hc.append(V(ht[:, k * TT:(k + 1) * TT], [T()]))
        self.H.ts = [c.ts[0] for c in self.Hc]
        self.VF = self.sb("VF", [128, NCH * TT], BF16)
        self.VFc = [V(self.VF.ap[:, k * TT:(k + 1) * TT], [T()]) for k in range(NCH)]
        self.rstd = [self.sb(f"rstd{i}", [128, TT], F32) for i in range(2)]
        self.rstd_i = 0
        self.PT = self.sb("PT", [128, 2 * TT], BF16)
        self.ST = [self.sb(f"ST{j}", [128, NCH * 128], BF16) for j in range(2)]
        self.xprev = [self.sb(f"xprev{j}", [128, NCH], F32) for j in range(2)]
        self.PC = self.sb("PC", [128, NCH * 4], F32)
        self.PCX = self.sb("PCX", [128, NCH * 8], F32)
        self.small = self.sb("small", [128, 64], F32)
        self.TW = self.sb("TW", [128, 256], BF16)
        self.TA = self.sb("TA", [128, 256], BF16)
        self.TV = self.sb("TV", [128, 256], BF16)
        self.TG1 = self.sb("TG1", [128, 256], BF16)
        self.TG2 = self.sb("TG2", [128, 256], BF16)
        for s in ("x", "p", "y", "c0", "c1"):
            fw.new_sem("io_" + s)
        self.NCS = 8
        self.ctok = []
        for i in range(self.NCS):
            fw.new_sem(f"cast{i}")
            self.ctok.append(T())
        fw.dma("sp", "io_c0", self.P1.ap, self.P1d, writes=self.P1.ts)
        fw.dma("pool", "io_c1", self.CSTb.ap, self.CSTd, writes=self.CSTb.ts)
        for j in range(2):
            self.op("dve", "tensor_scalar", self.pc(f"omka{j}", 0, 8), self.pc(f"ka{j}", 0, 8), -1.0, 1.0,
                    ALU.mult, ALU.add)
        for j in range(2):
            self.op("dve", "memset", self.ST[j], 0.0)
            self.op("dve", "memset", self.xprev[j], 0.0)
        ci = 0
        fw.dma("pool", f"cast{ci}", self.Pb, self.pT, writes=[self.pbT, self.ctok[ci]])
        ci += 1
        self.cast_i = 0
        self.cast_ci = ci
        self.ws = WStream(self, self.names, self.offs, self.ntiles)

    def ensure_cast(self, upto):
        while self.cast_i < min(upto, len(self.names)):
            n = self.names[self.cast_i]
            off, k = self.offs[n]
            s = self.cast_ci % self.NCS
            self.fw.dma("pool", f"cast{s}", self.Wb[:, off:off + k], self.W[:, off:off + k],
                        writes=[self.wbT[n], self.ctok[s]])
            self.cast_ci += 1
            self.cast_i += 1

    def rms_stats(self, src, sq, mat, epsname, n=TT):
        for k in range(NCH):
            self.op("act", "activation", sq[k], src[k], AF.Square)
        b = self.bank()
        bo = b[:, 0:n]
        self.mm(bo, [(self.cm(mat), sq[k]) for k in range(NCH)])
        r = self.rstd[self.rstd_i][:, 0:n]
        self.rstd_i ^= 1
        self.op("act", "activation", r, bo, AF.Ln, bias=self.pc(epsname), scale=1.0)
        self.op("act", "activation", r, r, AF.Exp, scale=-0.5)
        return r

    def gcol(self, l, i, k):
        return self.pc("g", (l * 8 + i) * 8 + k)

    def norm_to(self, src, outs, l, i, rstd):
        for k in range(NCH):
            self.op("dve", "scalar_tensor_tensor", outs[k], src[k], self.gcol(l, i, k), rstd, ALU.mult, ALU.mult)

    def evac_out(self, c, ps, M, SQ, l, i):
        self.op("act", "activation", SQ[c], ps, AF.Square)
        self.op("dve", "tensor_scalar_mul", M[c], ps, self.gcol(l, i, c))

    def finish_out(self, M, SQ, mat, epsname, n=TT, hsl=None):
        b = self.bank()
        bo = b[:, 0:n]
        self.mm(bo, [(self.cm(mat), SQ[k]) for k in range(NCH)])
        r = self.rstd[self.rstd_i][:, 0:n]
        self.rstd_i ^= 1
        self.op("act", "activation", r, bo, AF.Ln, bias=self.pc(epsname), scale=1.0)
        self.op("act", "activation", r, r, AF.Exp, scale=-0.5)
        for k in range(NCH):
            h = self.Hc[k] if hsl is None else self.Hc[k][:, hsl[0]:hsl[1]]
            self.op("dve", "tensor_tensor", M[k], M[k], r, ALU.mult)
            self.op("pool", "tensor_tensor", h, h, M[k], ALU.add)

    def add_to_h(self, M, l, i, rstd, hsl=None):
        for k in range(NCH):
            h = self.Hc[k] if hsl is None else self.Hc[k][:, hsl[0]:hsl[1]]
            self.op("dve", "scalar_tensor_tensor", M[k], M[k], self.gcol(l, i, k), rstd, ALU.mult, ALU.mult)
            self.op("dve", "tensor_tensor", h, h, M[k], ALU.add)

    def ffn(self, l, i):
        XN = Role(self, 0, 8, BF16).chunks(8, TT)
        SQ = Role(self, 8, 8, BF16).chunks(8, TT)
        ACTT = Role(self, 16, 22, BF16).chunks(NFC, TT)
        M = Role(self, 38, 16, F32).chunks(8, TT)
        SG = Role(self, 54, 4, F32).chunks(2, TT)
        rstd = self.rms_stats(self.Hc, SQ, "ones_mean", "eps_rms")
        self.norm_to(self.Hc, XN, l, 4 * i, rstd)
        for b in range(11):
            w = self.ws.get(f"L{l}.f{i}.w13.{b}")
            wv = w.re("p (k g c) -> p k g c", k=8, g=2, c=256)
            for sub in range(2):
                fc = 2 * b + sub
                pg = self.bank()
                pu = self.bank()
                self.mm(pg, [(wv[:, k, 0, sub * 128:(sub + 1) * 128], XN[k]) for k in range(NCH)])
                self.mm(pu, [(wv[:, k, 1, sub * 128:(sub + 1) * 128], XN[k]) for k in range(NCH)])
                sg = SG[fc % 2]
                self.op("act", "activation", sg, pg, AF.Silu)
                self.op("dve", "tensor_tensor", ACTT[fc], sg, pu, ALU.mult)
        for c in range(8):
            w = self.ws.get(f"L{l}.f{i}.w2.{c}")
            wv = w[:, 0:NFC * 128].re("p (f c) -> p f c", f=NFC, c=128)
            po = self.bank()
            self.mm(po, [(wv[:, fc, :], ACTT[fc]) for fc in range(NFC)])
            self.op("act", "copy", M[c], po)
        rstd = self.rms_stats(M, SQ, "ones_q", "eps_rms4")
        self.add_to_h(M, l, 4 * i + 1, rstd)

    def ple(self, l, t):
        XN = Role(self, 0, 8, BF16).chunks(8, TT)
        SQ = Role(self, 8, 8, BF16).chunks(8, TT)
        M = Role(self, 38, 16, F32).chunks(8, TT)
        SG = Role(self, 54, 4, F32).chunks(2, TT)
        self.fw.dma("sp", "io_p", self.PT.ap.rearrange("p (k t) -> p k t", k=2),
                    self.Pb[:, l, :, t * TT:(t + 1) * TT], reads=[self.pbT], writes=self.PT.ts)
        rstd = self.rms_stats(self.Hc, SQ, "ones_mean", "eps_rms")
        self.norm_to(self.Hc, XN, l, 6, rstd)
        wp = self.ws.get(f"L{l}.p.wp", hold=True).re("p (k c) -> p k c", k=2, c=1024)
        for q in range(2):
            wg = self.ws.get(f"L{l}.p.wg.{q}").re("p (k c) -> p k c", k=8, c=512)
            for c4 in range(4):
                c = 4 * q + c4
                bg = self.bank()
                bp = self.bank()
                self.mm(bg, [(wg[:, k, c4 * 128:(c4 + 1) * 128], XN[k]) for k in range(NCH)])
                self.mm(bp, [(wp[:, k2, c * 128:(c + 1) * 128], self.PT[:, k2 * TT:(k2 + 1) * TT]) for k2 in range(2)])
                sg = SG[c % 2]
                self.op("act", "activation", sg, bg, AF.Sigmoid)
                self.op("dve", "tensor_tensor", M[c], sg, bp, ALU.mult)
        self.ws.release(f"L{l}.p.wp")
        rstd = self.rms_stats(M, SQ, "ones_mean", "eps_rms")
        self.add_to_h(M, l, 7, rstd)

    def gmlp(self, l):
        j = l // 2
        XN = Role(self, 0, 8, BF16).chunks(8, TT)
        SQ = Role(self, 8, 8, BF16).chunks(8, TT)
        UTr = Role(self, 16, 16, BF16)
        UT = UTr.chunks(16, TT)
        M = Role(self, 38, 16, F32).chunks(8, TT)
        VTOK = Role(self, 54, 32, F32).chunks(4, 2048)
        VN = Role(self, 86, 8, BF16).chunks(2, 2048)
        rstd = self.rms_stats(self.Hc, SQ, "ones_mean", "eps_rms")
        self.norm_to(self.Hc, XN, l, 2, rstd)
        for q in range(4):
            w = self.ws.get(f"L{l}.g.wu.{q}").re("p (k c) -> p k c", k=8, c=512)
            for c4 in range(4):
                ec = 4 * q + c4
                b = self.bank()
                self.mm(b, [(w[:, k, c4 * 128:(c4 + 1) * 128], XN[k]) for k in range(NCH)])
                self.op("act", "activation", UT[ec], b, AF.Gelu, bias=self.pc(f"binu{j}", ec), scale=1.0)
        rows = self.ws.get(f"L{l}.g.rows", hold=True)
        ones_row = self.cm("ones1", 0, 1, 0, 128)
        for es in range(4):
            w = self.ws.get(f"L{l}.g.wv.{es}").re("p (k c) -> p k c", k=8, c=512)
            for tb in range(4):
                b = self.bank()
                terms = [(XN[k][:, tb * 128:(tb + 1) * 128], w[:, k, :]) for k in range(NCH)]
                terms.append((ones_row, rows[0:1, es * 512:(es + 1) * 512]))
                self.mm(b, terms)
                self.op("act", "activation", VTOK[tb][:, es * 512:(es + 1) * 512], b, AF.Gelu)
        vg = self.ws.get(f"L{l}.g.vg", hold=True)
        wsb = self.ws.get(f"L{l}.g.ws", hold=True)
        wsv = wsb[:, 0:1024].re("p (g i) -> p g i", g=8, i=128)
        self.op("dve", "memset", wsv[64:128, :, 0:64], 0.0)
        sm = self.small
        for tb in range(4):
            for c in range(4):
                self.op("dve", "bn_stats", sm[:, c * 6:(c + 1) * 6], VTOK[tb][:, c * 512:(c + 1) * 512])
            self.op("dve", "bn_aggr", sm[:, 24:26], sm[:, 0:24].re("p (c d) -> p c d", d=6))
            self.op("act", "activation", sm[:, 26:27], sm[:, 25:26], AF.Sqrt, bias=self.pc("eps_ln"), scale=1.0)
            self.op("dve", "reciprocal", sm[:, 26:27], sm[:, 26:27])
            self.op("dve", "scalar_tensor_tensor", sm[:, 27:28], sm[:, 24:25], -1.0, sm[:, 26:27], ALU.mult, ALU.mult)
            self.op("act", "activation", VTOK[tb], VTOK[tb], AF.Identity, bias=sm[:, 27:28], scale=sm[:, 26:27])
            vn = VN[tb % 2]
            self.op("dve", "tensor_tensor", vn, VTOK[tb], vg[:, 0:2048], ALU.mult)
            for q in range(4):
                b = self.bank()
                for c4 in range(4):
                    ec = 4 * q + c4
                    gi = ec // 2
                    bo = b[:, c4 * 128:(c4 + 1) * 128]
                    self.mm(bo, [(vn[:, ec * 128:(ec + 1) * 128], wsv[:, gi, :]),
                                 (ones_row, rows[0:1, 2048 + gi * 128:2048 + (gi + 1) * 128])])
                uv = V(UTr.ap.rearrange("p (e t) -> p e t", e=16, t=TT)[:, 4 * q:4 * q + 4, tb * 128:(tb + 1) * 128],
                       [UT[4 * q + c4].ts[0] for c4 in range(4)])
                self.op("dve", "tensor_tensor", uv, b.re("p (c i) -> p c i", c=4, i=128), uv, ALU.mult)
        for nm in ("rows", "vg", "ws"):
            self.ws.release(f"L{l}.g.{nm}")
        for q in range(4):
            w = self.ws.get(f"L{l}.g.wo.{q}").re("p (e c) -> p e c", e=16, c=256)
            for c2 in range(2):
                c = 2 * q + c2
                b = self.bank()
                self.mm(b, [(w[:, ec, c2 * 128:(c2 + 1) * 128], UT[ec]) for ec in range(16)])
                self.op("act", "copy", M[c], b)
        rstd = self.rms_stats(M, SQ, "ones_mean", "eps_rms")
        self.add_to_h(M, l, 3, rstd)


    def rwkv(self, l, t):
        for s in range(2):
            self.rwkv_sub(l, t, s)

    def rwkv_sub(self, l, t, s):
        j = l // 2
        TR = 256
        c0 = s * TR
        R = lambda u0, nu, dt=BF16: Role(self, u0, nu, dt)
        X, DX = R(0, 4), R(4, 4)
        XM = [R(8, 4), R(12, 4)]
        rK, rKK, rA, rR, rV, SQ = R(16, 4), R(20, 4), R(24, 4), R(28, 4), R(32, 4), R(36, 4)
        F3, F4 = R(40, 8, F32), R(48, 8, F32)
        AR, BK = R(56, 8), R(64, 8)
        ET = R(72, 3, F32).chunks(3, TR)
        A_ = [R(75, 2), R(77, 2)]
        N_ = [R(79, 2), R(81, 2)]
        T_ = [R(83, 2), R(85, 2)]
        GM = [R(91, 4), R(95, 4)]
        UV = [R(99, 2), R(101, 2)]
        BKT = [R(103, 2), R(105, 2)]
        RHST = R(107, 2)
        VT = [R(109, 2), R(111, 2)]
        SN = R(113, 2)
        TF = [R(87, 2), R(89, 2)]
        W1, W2 = R(75, 8, F32), R(91, 8, F32)
        Xc, DXc = X.chunks(8, TR), DX.chunks(8, TR)
        XMc = [XM[0].chunks(8, TR), XM[1].chunks(8, TR)]
        rKc, rKKc, rAc, rRc, rVc, SQc = (r.chunks(8, TR) for r in (rK, rKK, rA, rR, rV, SQ))
        F3c, F4c = F3.chunks(8, TR), F4.chunks(8, TR)
        b3 = lambda role: role.v().re("p (k t) -> p k t", k=8, t=TR)
        b4 = lambda role: role.v().re("p (k c x) -> p k c x", k=8, c=4, x=64)
        AR4 = AR.v().re("p (k c x) -> p k c x", k=8, c=4, x=128)
        BK4 = BK.v().re("p (k c x) -> p k c x", k=8, c=4, x=128)
        AR3 = [AR.v(c * 512, (c + 1) * 512).re("p (tc x) -> p tc x", tc=4, x=128) for c in range(8)]
        BK3 = [BK.v(c * 512, (c + 1) * 512).re("p (tc x) -> p tc x", tc=4, x=128) for c in range(8)]
        Hs = [self.Hc[k][:, c0:c0 + TR] for k in range(NCH)]
        ident = self.cm("ident")
        blk1 = self.cm("blk_1")
        blkg = self.cm("blk_gn")

        def pb(name):
            c = self.p1c[name]
            return self.P1[:, c:c + 8].re("p (k o) -> p k o", k=8, o=1).bc([128, 8, TR])

        def psum_wide(n):
            while self.bank_i % n:
                self.bank_i = (self.bank_i + 1) % 8
            i = self.bank_i
            self.bank_i = (self.bank_i + n) % 8
            ts = []
            for q in range(n):
                ts += self.banks[i + q].ts
            return V(self.psum[:, i * 512:(i + n) * 512], ts), [self.banks[i + q] for q in range(n)]

        rstd = self.rms_stats(Hs, SQc, "ones_mean", "eps_rms", n=TR)
        self.norm_to(Hs, Xc, l, 2, rstd)
        X3, DX3 = b3(X), b3(DX)
        xp3 = self.xprev[j].re("p (k o) -> p k o", k=8, o=1)
        self.op("dve", "tensor_tensor", DX3[:, :, 1:TR], X3[:, :, 0:TR - 1], X3[:, :, 1:TR], ALU.subtract)
        self.op("dve", "tensor_tensor", DX3[:, :, 0:1], xp3, X3[:, :, 0:1], ALU.subtract)
        self.op("act", "copy", xp3, X3[:, :, TR - 1:TR])

        def mix(m, dst):
            for k in range(NCH):
                self.op("dve", "scalar_tensor_tensor", dst[k], DXc[k], self.pc(f"mu{j}", m * 8 + k), Xc[k],
                        ALU.mult, ALU.add)
            return dst

        la = self.ws.get(f"L{l}.r.la.{s}", hold=True)
        lb = self.ws.get(f"L{l}.r.lb.{s}", hold=True)
        w1 = la[:, 0:512].re("p (k c) -> p k c", k=8, c=64)
        a1 = la[:, 512:1024].re("p (k c) -> p k c", k=8, c=64)
        g1 = la[:, 1024:2304].re("p (k c) -> p k c", k=8, c=160)
        v1 = la[:, 2304:2560].re("p (k c) -> p k c", k=8, c=32)
        w2, a2, v2 = lb[:, 0:1024], lb[:, 1024:2048], lb[:, 2048:3072]

        def proj(n, xm, outc):
            for q in range(2):
                w = self.ws.get(f"L{l}.r.win{n}.{q}.{s}").re("p (k c) -> p k c", k=8, c=512)
                for c4 in range(4):
                    c = 4 * q + c4
                    bb = self.bank()
                    self.mm(bb[:, 0:TR], [(w[:, k, c4 * 128:(c4 + 1) * 128], xm[k]) for k in range(NCH)])
                    self.op("act", "copy", outc[c], bb[:, 0:TR])

        xw = mix(3, XMc[0])
        b = self.bank()
        self.mm(b[0:64, 0:TR], [(w1[:, k, :], xw[k]) for k in range(NCH)])
        self.op("act", "activation", self.TW[0:64, 0:TR], b[0:64, 0:TR], AF.Tanh)
        for c in range(8):
            if c % 2 == 0:
                b = self.bank()
            bo = b[:, (c % 2) * TR:(c % 2 + 1) * TR]
            self.mm(bo, [(w2[0:64, c * 128:(c + 1) * 128], self.TW[0:64, 0:TR])])
            self.op("act", "activation", F3c[c], bo, AF.Sigmoid, bias=self.pc(f"w0{j}", c), scale=1.0)
        xa = mix(4, XMc[1])
        b = self.bank()
        self.mm(b[0:64, 0:TR], [(a1[:, k, :], xa[k]) for k in range(NCH)])
        self.op("act", "copy", self.TA[0:64, 0:TR], b[0:64, 0:TR])
        for c in range(8):
            if c % 2 == 0:
                b = self.bank()
            bo = b[:, (c % 2) * TR:(c % 2 + 1) * TR]
            self.mm(bo, [(a2[0:64, c * 128:(c + 1) * 128], self.TA[0:64, 0:TR])])
            self.op("act", "activation", rAc[c], bo, AF.Sigmoid, bias=self.pc(f"a0{j}", c), scale=1.0)
        xk = mix(1, XMc[0])
        proj(1, xk, rKc)
        src = F3.v().re("p (g t) -> p g t", t=64)
        dst = F4.v().re("p (g t) -> p g t", t=64)
        for sft in (1, 2, 4, 8, 16, 32):
            self.op("dve", "tensor_tensor", dst[:, :, sft:64], src[:, :, sft:64], src[:, :, 0:64 - sft], ALU.add)
            self.op("act", "copy", dst[:, :, 0:sft], src[:, :, 0:sft])
            src, dst = dst, src
        self.op("act", "activation", F4.v(), F3.v(), AF.Exp, scale=-C0)
        self.op("act", "activation", F3.v(), F3.v(), AF.Exp, scale=C0)
        PC3 = self.PC.re("p (k c) -> p k c", k=8, c=4)
        self.op("dve", "tensor_copy", PC3, F4.v().re("p (k c t) -> p k c t", k=8, c=4, t=64)[:, :, :, 63])
        PCX = self.PCX.re("p (k e c) -> p k e c", k=8, e=2, c=4)
        self.op("act", "copy", PCX[0:64, :, 0, :], PC3[0:64, :, :])
        self.op("dve", "tensor_copy", PCX[0:64, :, 1, :], PC3[64:128, :, :])

        xr = mix(0, XMc[1])
        proj(0, xr, rRc)
        xv = mix(2, XMc[0])
        proj(2, xv, rVc)
        if j >= 1:
            b = self.bank()
            self.mm(b[0:32, 0:TR], [(v1[:, k, :], xv[k]) for k in range(NCH)])
            self.op("act", "copy", self.TV[0:32, 0:TR], b[0:32, 0:TR])

        K3, KK3, A3, R3, V3, SQ3 = b3(rK), b3(rKK), b3(rA), b3(rR), b3(rV), b3(SQ)
        E1_4, E3_4 = b4(F4), b4(F3)
        w1v, w2v = W1.v(), W2.v()
        w1_3 = w1v.re("p (k t) -> p k t", k=8, t=TR)
        w2_3 = w2v.re("p (k t) -> p k t", k=8, t=TR)
        w1_4 = w1v.re("p (k c x) -> p k c x", k=8, c=4, x=64)
        self.op("dve", "tensor_tensor", KK3, K3, pb(f"kk{j}"), ALU.mult)
        self.op("act", "activation", SQ3, KK3, AF.Square)
        pw, _ = psum_wide(4)
        for c in range(8):
            self.mm(pw[:, c * TR:(c + 1) * TR], [(blk1, SQc[c])])
        self.op("dve", "tensor_scalar_max", w1v, pw, 1e-24)
        self.op("act", "activation", w1v, w1v, AF.Ln)
        self.op("act", "activation", w1v, w1v, AF.Exp, scale=-0.5)
        self.op("dve", "tensor_tensor", KK3, KK3, w1_3, ALU.mult)
        self.op("dve", "tensor_tensor", w2_3, A3, pb(f"ka{j}"), ALU.mult)
        self.op("dve", "tensor_tensor", w2_3, w2_3, pb(f"omka{j}"), ALU.add)
        self.op("dve", "tensor_tensor", K3, K3, w2_3, ALU.mult)
        self.op("dve", "tensor_tensor", w1_3, KK3, A3, ALU.mult)
        self.op("pool", "tensor_tensor", BK4[:, :, :, 0:64], w1_4, E3_4, ALU.mult)
        self.op("pool", "tensor_tensor", BK4[:, :, :, 64:128], b4(rK), E3_4, ALU.mult)
        self.op("dve", "scalar_tensor_tensor", AR4[:, :, :, 1:64], b4(rKK)[:, :, :, 1:64], -1.0,
                E1_4[:, :, :, 0:63], ALU.mult, ALU.mult)
        self.op("act", "mul", AR4[:, :, :, 0:1], b4(rKK)[:, :, :, 0:1], -1.0)
        self.op("pool", "tensor_tensor", AR4[:, :, :, 64:128], b4(rR), E1_4, ALU.mult)
        self.op("dve", "tensor_tensor", SQ3, R3, pb(f"rk{j}"), ALU.mult)
        self.op("dve", "tensor_tensor", SQ3, SQ3, K3, ALU.mult)
        VFs = V(self.VF.ap.rearrange("p (k t) -> p k t", k=8, t=TT)[:, :, c0:c0 + TR], [c.ts[0] for c in self.VFc])
        if j >= 1:
            pw, _ = psum_wide(4)
            for c in range(8):
                self.mm(pw[:, c * TR:(c + 1) * TR], [(v2[0:32, c * 128:(c + 1) * 128], self.TV[0:32, 0:TR])])
                self.op("act", "activation", w2v[:, c * TR:(c + 1) * TR], pw[:, c * TR:(c + 1) * TR], AF.Sigmoid,
                        bias=self.pc(f"v0{j}", c), scale=1.0)
            self.op("dve", "tensor_tensor", w1_3, VFs, V3, ALU.subtract)
            self.op("dve", "tensor_tensor", w1_3, w1_3, w2_3, ALU.mult)
            self.op("dve", "tensor_tensor", V3, V3, w1_3, ALU.add)
        else:
            self.op("pool", "tensor_copy", VFs, V3)
        pw, _ = psum_wide(4)
        for c in range(8):
            self.mm(pw[:, c * TR:(c + 1) * TR], [(blk1, SQc[c])])
        self.op("dve", "tensor_tensor", A3, pw.re("p (k t) -> p k t", k=8, t=TR), V3, ALU.mult)

        xg = mix(5, XMc[1])
        lc = self.ws.get(f"L{l}.r.lc.{s}", hold=True)
        g2a, g2b = lc[:, 0:1024], lc[:, 1024:2048]
        b1 = self.bank()
        self.mm(b1[:, 0:TR], [(g1[:, k, 0:128], xg[k]) for k in range(NCH)])
        b2 = self.bank()
        self.mm(b2[0:32, 0:TR], [(g1[:, k, 128:160], xg[k]) for k in range(NCH)])
        self.op("act", "activation", self.TG1[:, 0:TR], b1[:, 0:TR], AF.Sigmoid)
        self.op("act", "activation", self.TG2[0:32, 0:TR], b2[0:32, 0:TR], AF.Sigmoid)
        for c in range(8):
            bb = self.bank()
            self.mm(bb[:, 0:TR], [(g2a[:, c * 128:(c + 1) * 128], self.TG1[:, 0:TR]),
                                  (g2b[0:32, c * 128:(c + 1) * 128], self.TG2[0:32, 0:TR])])
            self.op("act", "copy", rKKc[c], bb[:, 0:TR])
        for nm in ("la", "lb", "lc"):
            self.ws.release(f"L{l}.r.{nm}.{s}")

        ST4 = self.ST[j].re("p (k h i) -> p k h i", k=8, h=2, i=64)
        Y4 = F4.v().re("p (k c t) -> p k c t", k=8, c=4, t=64)
        maskg = self.cm("maskg4", 0, 128, 0, 512)
        maskn = self.cm("maskn16", 0, 64, 0, 1024)
        id16 = self.cm("ident16", 0, 64, 0, 1024)
        for st in range(2):
            self.op("dve", "memset", VT[st].v()[0:64, :], 0.0)

        def hs_(h):
            return slice(h * 64, (h + 1) * 64)

        def tphase(tc):
            st = tc % 2
            gm4 = GM[st].v().re("p (d e x) -> p d e x", d=8, e=2, x=128)
            gm3 = GM[st].v().re("p (h x) -> p h x", h=16, x=128)
            for hf in range(2):
                bt = self.bank()
                for d4 in range(4):
                    dc = 4 * hf + d4
                    self.op("pe", "matmul", bt[:, d4 * 128:(d4 + 1) * 128], BK3[dc][:, tc, :], ident,
                            start=True, stop=True, inc=(d4 == 3))
                self.op("act", "copy", BKT[st].v()[:, hf * 512:(hf + 1) * 512], bt)
            for hf in range(2):
                bv = self.bank()
                for d4 in range(4):
                    dc = 4 * hf + d4
                    self.op("pe", "matmul", bv[0:64, d4 * 128:(d4 + 1) * 128], rVc[dc][:, tc * 64:(tc + 1) * 64], ident,
                            start=True, stop=True, inc=(d4 == 3))
                self.op("dve", "tensor_copy", VT[st].v()[64:128, hf * 512:(hf + 1) * 512], bv[0:64, :])
            yield
            for hp in range(2):
                ps = slice(64 * hp, 64 * hp + 64)
                for q in range(2):
                    bg = self.bank()
                    for d4 in range(4):
                        dc = 4 * q + d4
                        self.op("pe", "matmul", bg[:, d4 * 128:(d4 + 1) * 128], BK3[dc][ps, tc, :],
                                AR3[dc][ps, tc, :], start=True, stop=True, inc=(d4 == 3))
                    self.op("dve", "tensor_tensor", gm4[:, 4 * q:4 * q + 4, hp, :],
                            bg.re("p (d x) -> p d x", d=4, x=128),
                            maskg.re("p (d x) -> p d x", d=4, x=128), ALU.mult)
            n4 = N_[0].v()[0:64, :].re("p (d e x) -> p d e x", d=8, e=2, x=64)
            for hp in range(2):
                ps = slice(64 * hp, 64 * hp + 64)
                bn1 = self.bank()
                for dc in range(8):
                    self.op("pe", "matmul", bn1[0:64, dc * 64:(dc + 1) * 64],
                            AR3[dc][ps, tc, 0:64], BK3[dc][ps, tc, 0:64], start=True, stop=True, inc=(dc == 7))
                self.op("dve", "tensor_tensor", n4[:, :, hp, :], bn1[0:64, :].re("p (d x) -> p d x", d=8, x=64),
                        maskn[:, 0:512].re("p (d x) -> p d x", d=8, x=64), ALU.mult)
            self.op("act", "copy", A_[0].v()[0:64, :].re("p (h x) -> p h x", h=16, x=64), gm3[0:64, :, 0:64])
            self.op("dve", "tensor_tensor", T_[0].v()[0:64, :], A_[0].v()[0:64, :], id16, ALU.add)
            yield
            cur, tcur = 0, 0
            for step in range(1, 7):
                nxt = 1 - cur
                Ac, Nc = A_[cur].v()[0:64, :], N_[cur].v()[0:64, :]
                An, Nn = A_[nxt].v()[0:64, :], N_[nxt].v()[0:64, :]
                Tc, Tn = T_[tcur].v()[0:64, :], T_[1 - tcur].v()[0:64, :]
                if step == 6:
                    Tn = TF[st].v()[0:64, :]
                do_sq = step <= 5
                do_a = step <= 4
                do_t = step >= 2
                if do_a:
                    ba = [self.bank(), self.bank()]
                    for h in range(16):
                        self.op("pe", "matmul", ba[h // 8][0:64, hs_(h % 8)], Nc[:, hs_(h)], Ac[:, hs_(h)],
                                start=True, stop=True, inc=(h % 8 == 7))
                if do_sq:
                    bn = [self.bank(), self.bank()]
                    for h in range(16):
                        self.op("pe", "matmul", bn[h // 8][0:64, hs_(h % 8)], Ac[:, hs_(h)], Nc[:, hs_(h)],
                                start=True, stop=True, inc=(h % 8 == 7))
                if do_t:
                    bt2 = [self.bank(), self.bank()]
                    for h in range(16):
                        self.op("pe", "matmul", bt2[h // 8][0:64, hs_(h % 8)], Nc[:, hs_(h)], Tc[:, hs_(h)],
                                start=True, stop=True, inc=(h % 8 == 7))
                for hf in range(2):
                    fs = slice(hf * 512, (hf + 1) * 512)
                    if do_a:
                        self.op("act", "copy", An[:, fs], ba[hf][0:64, :])
                    if do_sq:
                        self.op("act" if not do_a else "dve", "copy" if not do_a else "tensor_copy", Nn[:, fs],
                                bn[hf][0:64, :])
                    if do_t:
                        self.op("dve", "tensor_tensor", Tn[:, fs], bt2[hf][0:64, :], Tc[:, fs], ALU.add)
                if do_sq:
                    cur = nxt
                if do_t:
                    tcur = 1 - tcur
                yield

        def chain(tc):
            st = tc % 2
            gm3 = GM[st].v().re("p (h x) -> p h x", h=16, x=128)
            uv3 = UV[st].v().re("p (h i) -> p h i", h=16, i=64)
            bkt3 = BKT[st].v().re("p (h i) -> p h i", h=16, i=64)
            vt3 = VT[st].v().re("p (h i) -> p h i", h=16, i=64)
            Tf = TF[st].v()[0:64, :]
            br = [self.bank(), self.bank()]
            for h in range(16):
                dc, hp = h // 2, h % 2
                out = br[h // 8][0:64, hs_(h % 8)]
                self.op("pe", "matmul", out, AR3[dc][:, tc, 0:64], ST4[:, dc, hp, :], start=True, stop=False, inc=False)
                self.op("pe", "matmul", out, gm3[:, h, 0:64], vt3[:, h, :], start=False, stop=True,
                        inc=(h % 8 == 7))
            self.op("act", "copy", RHST.v()[0:64, 0:512], br[0][0:64, :])
            self.op("dve", "tensor_copy", RHST.v()[0:64, 512:1024], br[1][0:64, :])
            yield
            bu = [self.bank(), self.bank()]
            for h in range(16):
                self.op("pe", "matmul", bu[h // 8][0:64, hs_(h % 8)], Tf[:, hs_(h)],
                        RHST.v()[0:64, hs_(h)], start=True, stop=True, inc=(h % 8 == 7))
            self.op("act", "copy", UV[st].v()[0:64, 0:512], bu[0][0:64, :])
            self.op("dve", "tensor_copy", UV[st].v()[0:64, 512:1024], bu[1][0:64, :])
            self.op("pool", "tensor_copy", UV[st].v()[64:128, :], VT[st].v()[64:128, :])
            yield
            bs = [self.bank(), self.bank()]
            for h in range(16):
                dc, hp = h // 2, h % 2
                out = bs[h // 8][0:64, hs_(h % 8)]
                self.op("pe", "matmul", out, bkt3[:, h, :], uv3[:, h, :], start=True, stop=False, inc=False)
                self.op("pe", "matmul", out, ident[:, 64 * hp:64 * hp + 64], ST4[:, dc, hp, :], start=False, stop=True,
                        inc=(h % 8 == 7))
            by = [self.bank(), self.bank()]
            for h in range(16):
                dc, hp = h // 2, h % 2
                out = by[h // 8][0:64, hs_(h % 8)]
                self.op("pe", "matmul", out, ST4[:, dc, hp, :], AR3[dc][:, tc, 64:128], start=True, stop=False,
                        inc=False)
                self.op("pe", "matmul", out, uv3[:, h, :], gm3[:, h, 64:128], start=False, stop=True,
                        inc=(h % 8 == 7))
            sn = SN.v()[0:64, :]
            for hf in range(2):
                self.op("dve", "tensor_tensor", sn[:, hf * 512:(hf + 1) * 512].re("p (d e i) -> p d e i", d=4, e=2, i=64),
                        bs[hf][0:64, :].re("p (d e i) -> p d e i", d=4, e=2, i=64),
                        PCX[0:64, 4 * hf:4 * hf + 4, :, tc:tc + 1].bc([64, 4, 2, 64]), ALU.mult)
            sn4 = sn.re("p (d e i) -> p d e i", d=8, e=2, i=64)
            self.op("act", "copy", ST4[0:64, :, 0, :], sn4[:, :, 0, :])
            self.op("dve", "tensor_copy", ST4[64:128, :, 1, :], sn4[:, :, 1, :])
            for hf in range(2):
                byv = by[hf][0:64, :].re("p (d e t) -> p d e t", d=4, e=2, t=64)
                self.op("act", "copy", Y4[0:64, 4 * hf:4 * hf + 4, tc, :], byv[:, :, 0, :])
                self.op("dve", "tensor_copy", Y4[64:128, 4 * hf:4 * hf + 4, tc, :], byv[:, :, 1, :])
            yield

        def drain(gen):
            for _ in gen:
                pass

        drain(tphase(0))
        for tc in range(4):
            gc = chain(tc)
            gt = tphase(tc + 1) if tc + 1 < 4 else iter(())
            alive_c, alive_t = True, True
            while alive_c or alive_t:
                if alive_t:
                    try:
                        next(gt)
                    except StopIteration:
                        alive_t = False
                if alive_t:
                    try:
                        next(gt)
                    except StopIteration:
                        alive_t = False
                if alive_c:
                    try:
                        next(gc)
                    except StopIteration:
                        alive_c = False

        Y3 = b3(F4)
        xo = XMc[0]
        self.op("act", "copy", SQ3, Y3)
        pw, _ = psum_wide(4)
        for c in range(8):
            self.mm(pw[:, c * TR:(c + 1) * TR], [(blkg, SQc[c])])
        self.op("dve", "tensor_tensor", Y3, Y3, pw.re("p (k t) -> p k t", k=8, t=TR), ALU.subtract)
        self.op("act", "activation", SQ3, Y3, AF.Square)
        pw, _ = psum_wide(4)
        for c in range(8):
            self.mm(pw[:, c * TR:(c + 1) * TR], [(blkg, SQc[c])])
        self.op("act", "activation", w1v, pw, AF.Ln, bias=self.pc("eps_gn"), scale=1.0)
        self.op("act", "activation", w1v, w1v, AF.Exp, scale=-0.5)
        self.op("dve", "tensor_tensor", Y3, Y3, w1_3, ALU.mult)
        self.op("dve", "tensor_tensor", Y3, Y3, pb(f"ln0{j}"), ALU.mult)
        self.op("pool", "tensor_tensor", Y3, Y3, pb(f"ln1{j}"), ALU.add)
        self.op("pool", "tensor_tensor", Y3, Y3, A3, ALU.add)
        self.op("dve", "tensor_tensor", b3(XM[0]), Y3, KK3, ALU.mult)
        Mc = F3c
        for q in range(2):
            w = self.ws.get(f"L{l}.r.wo.{q}.{s}").re("p (k c) -> p k c", k=8, c=512)
            for c4 in range(4):
                c = 4 * q + c4
                bb = self.bank()
                self.mm(bb[:, 0:TR], [(w[:, k, c4 * 128:(c4 + 1) * 128], xo[k]) for k in range(NCH)])
                self.op("act", "copy", Mc[c], bb[:, 0:TR])
        rstd = self.rms_stats(Mc, SQc, "ones_mean", "eps_rms", n=TR)
        self.add_to_h(Mc, l, 3, rstd, hsl=(c0, c0 + TR))

    def load_x(self, t):
        self.fw.dma("sp", "io_x", self.H.ap.rearrange("p (k t) -> p k t", k=NCH),
                    self.xT[:, :, t * TT:(t + 1) * TT], writes=self.H.ts)

    def store(self, t):
        self.fw.dma("sp", "io_y", self.yT[:, :, t * TT:(t + 1) * TT],
                    self.H.ap.rearrange("p (k t) -> p k t", k=NCH), reads=self.H.ts)

    def run(self, stages=None):
        self.setup()
        for t in range(self.ntiles):
            self.load_x(t)
            for l in range(self.nlayers):
                self.ffn(l, 0)
                if l % 2 == 0:
                    self.gmlp(l)
                else:
                    self.rwkv(l, t)
                self.ffn(l, 1)
                self.ple(l, t)
            self.store(t)
        self.fw.wait_all("sp", self.H.ts)
        self.fw.emit()


def build_nc(blocks_meta, ntiles, nlayers, s_core):
    nc = bass.Bass("TRN2", target_bir_lowering=False)
    with ExitStack() as st:
        g = Gen(nc, st, blocks_meta, ntiles, nlayers, s_core)
        g.run()
        n_ins = g.fw.n_ins
    return nc, n_ins


def prep_shared(inp, nlayers=4):
    blocks = block_list(inp, nlayers)
    meta = [(n, a.shape[1]) for n, a in blocks]
    W = np.concatenate([a for _, a in blocks], axis=1)
    return meta, W, build_p1(inp), build_cst()


def prep_core(x_b, p_b):
    S = x_b.shape[0]
    xT = np.ascontiguousarray(x_b.reshape(S, NCH, 128).transpose(2, 1, 0))
    pT = np.ascontiguousarray(p_b.reshape(4, S, 2, 128).transpose(3, 0, 2, 1))
    return xT, pT


def kernel(**inputs):
    inp = {k: np.asarray(v, np.float32) for k, v in inputs.items()}
    meta, W, P1, CSTa = prep_shared(inp)
    B = inp["x"].shape[0]
    nc, _ = build_nc(meta, SEQ // TT, 4, SEQ)
    in_maps = []
    for b in range(B):
        xT, pT = prep_core(inp["x"][b], inp["p"][:, b])
        in_maps.append({"xT": xT, "pT": pT, "W": W, "P1": P1, "CST": CSTa})
    res = run_bass_kernel_spmd(nc, in_maps, core_ids=list(range(B)))
    out = np.empty((B, SEQ, D), np.float32)
    for b in range(B):
        yT = res.results[b]["yT"]
        out[b] = yT.transpose(2, 1, 0).reshape(SEQ, D)
    return out
```

```python
import math
from contextlib import ExitStack

import numpy as np
import concourse.bass as bass
import concourse.mybir as mybir
from concourse.bass_utils import run_bass_kernel_spmd

F32 = mybir.dt.float32
BF16 = mybir.dt.bfloat16
AF = mybir.ActivationFunctionType
ALU = mybir.AluOpType
AX = mybir.AxisListType

D = 1024
NCH = 8
TT = 512
SEQ = 4096
DFF = 2816
NFC = 22
SLOT = 4096
RING = 5
C0 = math.exp(-0.5)


class T:
    __slots__ = ("w", "r")

    def __init__(self):
        self.w = None
        self.r = {}


class V:
    __slots__ = ("ap", "ts")

    def __init__(self, ap, ts):
        self.ap = ap
        self.ts = ts

    def __getitem__(self, idx):
        return V(self.ap[idx], self.ts)

    def re(self, pat, **kw):
        return V(self.ap.rearrange(pat, **kw), self.ts)

    def bc(self, shape):
        return V(self.ap.broadcast_to(list(shape)), self.ts)


class Eng:
    def __init__(self, name, same_wait):
        self.name = name
        self.items = []
        self.count = 0
        self.seen = {}
        self.same_wait = same_wait
        self.semkey = "E_" + name


class FW:
    def __init__(self, nc, stack):
        self.nc = nc
        self.stack = stack
        self.sems = {}
        self.engs = {}
        for name, sw in (("pe", False), ("act", True), ("dve", True), ("pool", True), ("sp", False)):
            e = Eng(name, sw)
            self.engs[name] = e
            self.sems[e.semkey] = stack.enter_context(nc.semaphore("s_" + name))
        self.dma_cnt = {}
        self.n_ins = 0

    def new_sem(self, key):
        self.sems[key] = self.stack.enter_context(self.nc.semaphore(key))
        self.dma_cnt[key] = 0
        return key

    def _waits(self, eng, reads, writes):
        need = {}

        def add(ev):
            if ev is None:
                return
            k, v = ev
            if k == eng.semkey and not eng.same_wait:
                return
            if eng.seen.get(k, 0) >= v:
                return
            if need.get(k, 0) < v:
                need[k] = v

        for t in reads:
            add(t.w)
        for t in writes:
            add(t.w)
            for k, v in t.r.items():
                add((k, v))
        for k, v in need.items():
            eng.seen[k] = v
            h = self.sems[k]
            if k == eng.semkey:
                assert v <= eng.count, "self-wait on pending event"
            eng.items.append(("w", h, v))

    def op(self, engname, meth, args, kw, reads, writes, inc=True):
        eng = self.engs[engname]
        self._waits(eng, reads, writes)
        self.n_ins += 1
        if inc:
            eng.count += 1
            eng.items.append(("i", meth, args, kw, self.sems[eng.semkey], 1))
            ev = (eng.semkey, eng.count)
        else:
            eng.items.append(("i", meth, args, kw, None, 0))
            ev = (eng.semkey, eng.count + 1)
        for t in reads:
            if t.r.get(ev[0], 0) < ev[1]:
                t.r[ev[0]] = ev[1]
        for t in writes:
            t.w = ev
            t.r = {}
        return ev

    def dma(self, engname, semkey, out, in_, reads=(), writes=()):
        eng = self.engs[engname]
        self._waits(eng, reads, writes)
        self.n_ins += 1
        self.dma_cnt[semkey] += 16
        eng.items.append(("i", "dma_start", (), {"out": out, "in_": in_}, self.sems[semkey], 16))
        ev = (semkey, self.dma_cnt[semkey])
        for t in reads:
            if t.r.get(ev[0], 0) < ev[1]:
                t.r[ev[0]] = ev[1]
        for t in writes:
            t.w = ev
            t.r = {}
        return ev

    def wait_all(self, engname, tiles):
        self._waits(self.engs[engname], (), tiles)

    def emit(self):
        def run(e, items):
            for it in items:
                if it[0] == "w":
                    e.wait_ge(it[1], it[2])
                else:
                    _, meth, args, kw, sem, n = it
                    ins = getattr(e, meth)(*args, **kw)
                    if sem is not None:
                        ins.then_inc(sem, n)

        with self.nc.Block() as block:
            @block.tensor
            def _(e):
                run(e, self.engs["pe"].items)

            @block.scalar
            def _(e):
                run(e, self.engs["act"].items)

            @block.vector
            def _(e):
                run(e, self.engs["dve"].items)

            @block.gpsimd
            def _(e):
                run(e, self.engs["pool"].items)

            @block.sync
            def _(e):
                run(e, self.engs["sp"].items)


def _kmaj(w):
    K, N = w.shape
    return w.reshape(K // 128, 128, N).transpose(1, 0, 2)


def _fm(v):
    return np.asarray(v, np.float32).reshape(-1, 128).T


def _pad_rows(a):
    out = np.zeros((128,) + a.shape[1:], np.float32)
    out[: a.shape[0]] = a
    return out


def block_list(inp, nlayers=4):
    blocks = []

    def add(name, arr):
        a = np.ascontiguousarray(np.asarray(arr, np.float32).reshape(128, -1))
        assert a.shape[1] <= SLOT, (name, a.shape)
        blocks.append((name, a))

    def ffn(l, i):
        w13k = _kmaj(inp["ffn_w13"][l, i])
        for b in range(11):
            g = w13k[:, :, 256 * b:256 * b + 256]
            u = w13k[:, :, DFF + 256 * b:DFF + 256 * b + 256]
            add(f"L{l}.f{i}.w13.{b}", np.stack([g, u], axis=2))
        w2k = _kmaj(inp["ffn_w2"][l, i])
        for c in range(8):
            add(f"L{l}.f{i}.w2.{c}", w2k[:, :, 128 * c:128 * c + 128])

    for l in range(nlayers):
        ffn(l, 0)
        j = l // 2
        if l % 2 == 0:
            win = _kmaj(inp["a_w_in"][j])
            for q in range(4):
                add(f"L{l}.g.wu.{q}", win[:, :, 512 * q:512 * q + 512])
            rows = np.zeros((128, 3072), np.float32)
            rows[0, :2048] = inp["a_b_in"][j][2048:]
            rows[0, 2048:] = inp["a_b_s"][j].reshape(-1)
            add(f"L{l}.g.rows", rows)
            for q in range(4):
                add(f"L{l}.g.wv.{q}", win[:, :, 2048 + 512 * q:2048 + 512 * q + 512])
            add(f"L{l}.g.vg", np.broadcast_to(inp["a_v_gain"][j][None, :], (128, 2048)))
            add(f"L{l}.g.ws", inp["a_w_s"][j].transpose(2, 0, 1))
            wo = _kmaj(inp["a_w_out"][j])
            for q in range(4):
                add(f"L{l}.g.wo.{q}", wo[:, :, 256 * q:256 * q + 256])
        else:
            la = np.concatenate([
                _kmaj(inp["b_w1"][j]).reshape(128, -1), _kmaj(inp["b_a1"][j]).reshape(128, -1),
                _kmaj(inp["b_g1"][j]).reshape(128, -1),
                (_kmaj(inp["b_v1"][j - 1]).reshape(128, -1) if j >= 1 else np.zeros((128, 256), np.float32))],
                axis=1)
            lb = np.concatenate([
                _pad_rows(inp["b_w2"][j]), _pad_rows(inp["b_a2"][j]),
                (_pad_rows(inp["b_v2"][j - 1]) if j >= 1 else np.zeros((128, 1024), np.float32))], axis=1)
            lc = np.concatenate([inp["b_g2"][j][:128], _pad_rows(inp["b_g2"][j][128:])], axis=1)
            wks = [_kmaj(inp["b_w_in"][j, n]) for n in range(3)]
            wo = _kmaj(inp["b_w_out"][j])
            for s in range(2):
                add(f"L{l}.r.la.{s}", la)
                add(f"L{l}.r.lb.{s}", lb)
                for n in (1, 0, 2):
                    for q in range(2):
                        add(f"L{l}.r.win{n}.{q}.{s}", wks[n][:, :, 512 * q:512 * q + 512])
                add(f"L{l}.r.lc.{s}", lc)
                for q in range(2):
                    add(f"L{l}.r.wo.{q}.{s}", wo[:, :, 512 * q:512 * q + 512])
        ffn(l, 1)
        add(f"L{l}.p.wp", _kmaj(inp["ple_w_proj"][l]))
        wg = _kmaj(inp["ple_w_gate"][l])
        for q in range(2):
            add(f"L{l}.p.wg.{q}", wg[:, :, 512 * q:512 * q + 512])
    return blocks


def p1_layout():
    cols = {}
    n = 0

    def add(name, k):
        nonlocal n
        cols[name] = n
        n += k

    add("g", 4 * 8 * 8)
    for j in range(2):
        add(f"binu{j}", 16)
    for j in range(2):
        add(f"mu{j}", 48)
        for nm in ("w0", "a0", "kk", "ka", "rk", "ln0", "ln1", "v0", "omka"):
            add(f"{nm}{j}", 8)
    for nm in ("eps_rms", "eps_rms4", "eps_ln", "eps_gn", "tiny"):
        add(nm, 1)
    return cols, n


def build_p1(inp):
    cols, n = p1_layout()
    p = np.zeros((128, n), np.float32)
    for l in range(4):
        for i in range(8):
            c = cols["g"] + (l * 8 + i) * 8
            p[:, c:c + 8] = _fm(inp["norm_g"][l, i])
    for j in range(2):
        p[:, cols[f"binu{j}"]:cols[f"binu{j}"] + 16] = _fm(inp["a_b_in"][j][:2048])
        for m in range(6):
            c = cols[f"mu{j}"] + m * 8
            p[:, c:c + 8] = _fm(inp["b_mu"][j, m])
        for nm, key in (("w0", "b_w0"), ("a0", "b_a0"), ("kk", "b_k_k"), ("ka", "b_k_a")):
            p[:, cols[f"{nm}{j}"]:cols[f"{nm}{j}"] + 8] = _fm(inp[key][j])
        p[:, cols[f"rk{j}"]:cols[f"rk{j}"] + 8] = _fm(inp["b_r_k"][j].reshape(-1))
        p[:, cols[f"ln0{j}"]:cols[f"ln0{j}"] + 8] = _fm(inp["b_lnx"][j, 0])
        p[:, cols[f"ln1{j}"]:cols[f"ln1{j}"] + 8] = _fm(inp["b_lnx"][j, 1])
        if j >= 1:
            p[:, cols[f"v0{j}"]:cols[f"v0{j}"] + 8] = _fm(inp["b_v0"][j - 1])
    p[:, cols["eps_rms"]] = 1e-6
    p[:, cols["eps_rms4"]] = 4e-6
    p[:, cols["eps_ln"]] = 1e-5
    p[:, cols["eps_gn"]] = 64e-5
    p[:, cols["tiny"]] = 1e-24
    return p


CST = {}


def cst_layout():
    cols = {}
    n = 0
    for name, k in (("ones_mean", 128), ("ones_q", 128), ("blk_gn", 128), ("blk_1", 128), ("ident", 128),
                    ("ones1", 128), ("maskg4", 512), ("maskn16", 1024), ("ident16", 1024)):
        cols[name] = n
        n += k
    return cols, n


def build_cst():
    cols, n = cst_layout()
    c = np.zeros((128, n), np.float32)
    c[:, cols["ones_mean"]:cols["ones_mean"] + 128] = 1.0 / 1024
    c[:, cols["ones_q"]:cols["ones_q"] + 128] = 1.0 / 256
    blk = np.zeros((128, 128), np.float32)
    blk[:64, :64] = 1
    blk[64:, 64:] = 1
    c[:, cols["blk_gn"]:cols["blk_gn"] + 128] = blk / 64
    c[:, cols["blk_1"]:cols["blk_1"] + 128] = blk
    c[:, cols["ident"]:cols["ident"] + 128] = np.eye(128)
    c[:, cols["ones1"]:cols["ones1"] + 128] = 1.0
    s = np.arange(64)
    strict = (s[:, None] < s[None, :]).astype(np.float32)
    incl = (s[:, None] <= s[None, :]).astype(np.float32)
    mg = np.concatenate([np.concatenate([strict, incl], 1)] * 2, 0)
    c[:, cols["maskg4"]:cols["maskg4"] + 512] = np.tile(mg, (1, 4))
    mn = (s[:, None] > s[None, :]).astype(np.float32)
    c[:64, cols["maskn16"]:cols["maskn16"] + 1024] = np.tile(mn, (1, 16))
    c[:64, cols["ident16"]:cols["ident16"] + 1024] = np.tile(np.eye(64, dtype=np.float32), (1, 16))
    return c


class Role:
    def __init__(self, g, u0, nu, dt):
        self.g = g
        self.u0 = u0
        self.nu = nu
        base = g.arena[:, u0 * 512:(u0 + nu) * 512]
        self.ap = base if dt == BF16 else base.bitcast(F32)
        self.epu = 512 if dt == BF16 else 256
        self.n = nu * self.epu

    def v(self, c0=0, c1=None):
        c1 = self.n if c1 is None else c1
        us = range(self.u0 + c0 // self.epu, self.u0 + (c1 - 1) // self.epu + 1)
        return V(self.ap[:, c0:c1], [self.g.units[u] for u in us])

    def chunks(self, n, w):
        return [self.v(i * w, (i + 1) * w) for i in range(n)]


class WStream:
    def __init__(self, g, names, offs, ntiles):
        self.g = g
        self.names = names
        self.offs = offs
        self.total = len(names) * ntiles
        self.issued = 0
        self.pos = 0
        self.slots = []
        for i in range(RING):
            t = g.stack.enter_context(g.nc.sbuf_tensor(f"ring{i}", [128, SLOT], BF16))
            self.slots.append(V(t[:], [T()]))
            g.fw.new_sem(f"ring{i}")
        self.free = list(range(RING))
        self.loaded = {}
        self.held = {}
        self.last = None

    def pump(self):
        while self.free and self.issued < self.total:
            s = self.free.pop(0)
            name = self.names[self.issued % len(self.names)]
            off, n = self.offs[name]
            g = self.g
            g.ensure_cast(self.issued + 1 + 16)
            g.fw.dma("sp", f"ring{s}", self.slots[s].ap[:, 0:n], g.Wb[:, off:off + n],
                     reads=[g.wbT[name]], writes=self.slots[s].ts)
            self.loaded[self.issued] = s
            self.issued += 1

    def get(self, name, hold=False):
        if self.last is not None:
            self.free.append(self.last)
            self.last = None
        self.pump()
        want = self.names[self.pos % len(self.names)]
        assert want == name, (want, name)
        s = self.loaded.pop(self.pos)
        self.pos += 1
        if hold:
            self.held[name] = s
        else:
            self.last = s
        return self.slots[s][:, 0:self.offs[name][1]]

    def release(self, name):
        self.free.append(self.held.pop(name))
        self.pump()


class Gen:
    def __init__(self, nc, stack, blocks_meta, ntiles, nlayers, s_core, debug_out=None):
        self.nc = nc
        self.stack = stack
        self.fw = FW(nc, stack)
        self.ntiles = ntiles
        self.nlayers = nlayers
        self.s_core = s_core
        names = [n for n, _ in blocks_meta]
        self.offs = {}
        off = 0
        for n, k in blocks_meta:
            self.offs[n] = (off, k)
            off += k
        self.ncols = off
        self.names = names
        self.p1c, self.np1 = p1_layout()
        self.cc, self.ncst = cst_layout()
        self.xT = nc.dram_tensor("xT", [128, NCH, s_core], F32, kind="ExternalInput").ap()
        self.pT = nc.dram_tensor("pT", [128, 4, 2, s_core], F32, kind="ExternalInput").ap()
        self.W = nc.dram_tensor("W", [128, self.ncols], F32, kind="ExternalInput").ap()
        self.P1d = nc.dram_tensor("P1", [128, self.np1], F32, kind="ExternalInput").ap()
        self.CSTd = nc.dram_tensor("CST", [128, self.ncst], F32, kind="ExternalInput").ap()
        self.yT = nc.dram_tensor("yT", [128, NCH, s_core], F32, kind="ExternalOutput").ap()
        self.Wb = nc.dram_tensor("Wb", [128, self.ncols], BF16, kind="Internal").ap()
        self.Pb = nc.dram_tensor("Pb", [128, 4, 2, s_core], BF16, kind="Internal").ap()
        self.wbT = {n: T() for n in names}
        self.pbT = T()

    def sb(self, name, shape, dt=F32):
        t = self.stack.enter_context(self.nc.sbuf_tensor(name, list(shape), dt))
        return V(t[:], [T()])

    def op(self, eng, meth, out, *args, inc=True, **kw):
        reads, writes = [], list(out.ts)
        a2 = [out.ap]
        for a in args:
            if isinstance(a, V):
                reads += a.ts
                a2.append(a.ap)
            else:
                a2.append(a)
        k2 = {}
        for k, v in kw.items():
            if isinstance(v, V):
                if k == "accum_out":
                    writes += v.ts
                else:
                    reads += v.ts
                k2[k] = v.ap
            else:
                k2[k] = v
        return self.fw.op(eng, meth, tuple(a2), k2, reads, writes, inc=inc)

    def mm(self, out, terms, tps=None):
        n = len(terms)
        for i, (l, r) in enumerate(terms):
            kw = {}
            if tps is not None and tps[i] is not None:
                kw["tile_position"] = tps[i]
            self.op("pe", "matmul", out, l, r, start=(i == 0), stop=(i == n - 1), inc=(i == n - 1), **kw)

    def bank(self):
        b = self.banks[self.bank_i]
        self.bank_i = (self.bank_i + 1) % 8
        return b

    def pc(self, name, k=0, n=1):
        c = self.p1c[name] + k
        return self.P1[:, c:c + n]

    def cm(self, name, r0=0, r1=128, c0=0, c1=128):
        c = self.cc[name]
        return self.CSTb[r0:r1, c + c0:c + c1]

    def setup(self):
        nc, st, fw = self.nc, self.stack, self.fw
        self.NU = 116
        at = st.enter_context(nc.sbuf_tensor("arena", [128, self.NU * 512], BF16))
        self.arena = at[:]
        self.units = [T() for _ in range(self.NU)]
        self.banks = []
        pt = st.enter_context(nc.psum_tensor("psum_all", [128, 8 * 512], F32))
        self.psum = pt[:]
        for i in range(8):
            self.banks.append(V(self.psum[:, i * 512:(i + 1) * 512], [T()]))
        self.bank_i = 0
        self.P1 = self.sb("P1s", [128, self.np1], F32)
        self.CSTb = self.sb("CSTs", [128, self.ncst], BF16)
        self.H = self.sb("H", [128, NCH * TT], F32)
        self.Hc = []
        ht = self.H.ap
        for k in range(NCH):
            self.Hc.append(V(ht[:, k * TT:(k + 1) * TT], [T()]))
        self.H.ts = [c.ts[0] for c in self.Hc]
        self.VF = self.sb("VF", [128, NCH * TT], BF16)
        self.VFc = [V(self.VF.ap[:, k * TT:(k + 1) * TT], [T()]) for k in range(NCH)]
        self.rstd = [self.sb(f"rstd{i}", [128, TT], F32) for i in range(2)]
        self.rstd_i = 0
        self.PT = self.sb("PT", [128, 2 * TT], BF16)
        self.ST = [self.sb(f"ST{j}", [128, NCH * 128], BF16) for j in range(2)]
        self.xprev = [self.sb(f"xprev{j}", [128, NCH], F32) for j in range(2)]
        self.PC = self.sb("PC", [128, NCH * 4], F32)
        self.PCX = self.sb("PCX", [128, NCH * 8], F32)
        self.small = self.sb("small", [128, 64], F32)
        self.TW = self.sb("TW", [128, 256], BF16)
        self.TA = self.sb("TA", [128, 256], BF16)
        self.TV = self.sb("TV", [128, 256], BF16)
        self.TG1 = self.sb("TG1", [128, 256], BF16)
        self.TG2 = self.sb("TG2", [128, 256], BF16)
        for s in ("x", "p", "y", "c0", "c1"):
            fw.new_sem("io_" + s)
        self.NCS = 8
        self.ctok = []
        for i in range(self.NCS):
            fw.new_sem(f"cast{i}")
            self.ctok.append(T())
        fw.dma("sp", "io_c0", self.P1.ap, self.P1d, writes=self.P1.ts)
        fw.dma("pool", "io_c1", self.CSTb.ap, self.CSTd, writes=self.CSTb.ts)
        for j in range(2):
            self.op("dve", "tensor_scalar", self.pc(f"omka{j}", 0, 8), self.pc(f"ka{j}", 0, 8), -1.0, 1.0,
                    ALU.mult, ALU.add)
        for j in range(2):
            self.op("dve", "memset", self.ST[j], 0.0)
            self.op("dve", "memset", self.xprev[j], 0.0)
        ci = 0
        fw.dma("pool", f"cast{ci}", self.Pb, self.pT, writes=[self.pbT, self.ctok[ci]])
        ci += 1
        self.cast_i = 0
        self.cast_ci = ci
        self.ws = WStream(self, self.names, self.offs, self.ntiles)

    def ensure_cast(self, upto):
        while self.cast_i < min(upto, len(self.names)):
            n = self.names[self.cast_i]
            off, k = self.offs[n]
            s = self.cast_ci % self.NCS
            self.fw.dma("pool", f"cast{s}", self.Wb[:, off:off + k], self.W[:, off:off + k],
                        writes=[self.wbT[n], self.ctok[s]])
            self.cast_ci += 1
            self.cast_i += 1

    def rms_stats(self, src, sq, mat, epsname, n=TT):
        for k in range(NCH):
            self.op("act", "activation", sq[k], src[k], AF.Square)
        b = self.bank()
        bo = b[:, 0:n]
        self.mm(bo, [(self.cm(mat), sq[k]) for k in range(NCH)])
        r = self.rstd[self.rstd_i][:, 0:n]
        self.rstd_i ^= 1
        self.op("act", "activation", r, bo, AF.Ln, bias=self.pc(epsname), scale=1.0)
        self.op("act", "activation", r, r, AF.Exp, scale=-0.5)
        return r

    def gcol(self, l, i, k):
        return self.pc("g", (l * 8 + i) * 8 + k)

    def norm_to(self, src, outs, l, i, rstd):
        for k in range(NCH):
            self.op("dve", "scalar_tensor_tensor", outs[k], src[k], self.gcol(l, i, k), rstd, ALU.mult, ALU.mult)

    def evac_out(self, c, ps, M, SQ, l, i):
        self.op("act", "activation", SQ[c], ps, AF.Square)
        self.op("dve", "tensor_scalar_mul", M[c], ps, self.gcol(l, i, c))

    def finish_out(self, M, SQ, mat, epsname, n=TT, hsl=None):
        b = self.bank()
        bo = b[:, 0:n]
        self.mm(bo, [(self.cm(mat), SQ[k]) for k in range(NCH)])
        r = self.rstd[self.rstd_i][:, 0:n]
        self.rstd_i ^= 1
        self.op("act", "activation", r, bo, AF.Ln, bias=self.pc(epsname), scale=1.0)
        self.op("act", "activation", r, r, AF.Exp, scale=-0.5)
        for k in range(NCH):
            h = self.Hc[k] if hsl is None else self.Hc[k][:, hsl[0]:hsl[1]]
            self.op("dve", "tensor_tensor", M[k], M[k], r, ALU.mult)
            self.op("pool", "tensor_tensor", h, h, M[k], ALU.add)

    def add_to_h(self, M, l, i, rstd, hsl=None):
        for k in range(NCH):
            h = self.Hc[k] if hsl is None else self.Hc[k][:, hsl[0]:hsl[1]]
            self.op("dve", "scalar_tensor_tensor", M[k], M[k], self.gcol(l, i, k), rstd, ALU.mult, ALU.mult)
            self.op("dve", "tensor_tensor", h, h, M[k], ALU.add)

    def ffn(self, l, i):
        XN = Role(self, 0, 8, BF16).chunks(8, TT)
        SQ = Role(self, 8, 8, BF16).chunks(8, TT)
        ACTT = Role(self, 16, 22, BF16).chunks(NFC, TT)
        M = Role(self, 38, 16, F32).chunks(8, TT)
        SG = Role(self, 54, 4, F32).chunks(2, TT)
        rstd = self.rms_stats(self.Hc, SQ, "ones_mean", "eps_rms")
        self.norm_to(self.Hc, XN, l, 4 * i, rstd)
        for b in range(11):
            w = self.ws.get(f"L{l}.f{i}.w13.{b}")
            wv = w.re("p (k g c) -> p k g c", k=8, g=2, c=256)
            for sub in range(2):
                fc = 2 * b + sub
                pg = self.bank()
                pu = self.bank()
                self.mm(pg, [(wv[:, k, 0, sub * 128:(sub + 1) * 128], XN[k]) for k in range(NCH)])
                self.mm(pu, [(wv[:, k, 1, sub * 128:(sub + 1) * 128], XN[k]) for k in range(NCH)])
                sg = SG[fc % 2]
                self.op("act", "activation", sg, pg, AF.Silu)
                self.op("dve", "tensor_tensor", ACTT[fc], sg, pu, ALU.mult)
        for c in range(8):
            w = self.ws.get(f"L{l}.f{i}.w2.{c}")
            wv = w[:, 0:NFC * 128].re("p (f c) -> p f c", f=NFC, c=128)
            po = self.bank()
            self.mm(po, [(wv[:, fc, :], ACTT[fc]) for fc in range(NFC)])
            self.op("act", "copy", M[c], po)
        rstd = self.rms_stats(M, SQ, "ones_q", "eps_rms4")
        self.add_to_h(M, l, 4 * i + 1, rstd)

    def ple(self, l, t):
        XN = Role(self, 0, 8, BF16).chunks(8, TT)
        SQ = Role(self, 8, 8, BF16).chunks(8, TT)
        M = Role(self, 38, 16, F32).chunks(8, TT)
        SG = Role(self, 54, 4, F32).chunks(2, TT)
        self.fw.dma("sp", "io_p", self.PT.ap.rearrange("p (k t) -> p k t", k=2),
                    self.Pb[:, l, :, t * TT:(t + 1) * TT], reads=[self.pbT], writes=self.PT.ts)
        rstd = self.rms_stats(self.Hc, SQ, "ones_mean", "eps_rms")
        self.norm_to(self.Hc, XN, l, 6, rstd)
        wp = self.ws.get(f"L{l}.p.wp", hold=True).re("p (k c) -> p k c", k=2, c=1024)
        for q in range(2):
            wg = self.ws.get(f"L{l}.p.wg.{q}").re("p (k c) -> p k c", k=8, c=512)
            for c4 in range(4):
                c = 4 * q + c4
                bg = self.bank()
                bp = self.bank()
                self.mm(bg, [(wg[:, k, c4 * 128:(c4 + 1) * 128], XN[k]) for k in range(NCH)])
                self.mm(bp, [(wp[:, k2, c * 128:(c + 1) * 128], self.PT[:, k2 * TT:(k2 + 1) * TT]) for k2 in range(2)])
                sg = SG[c % 2]
                self.op("act", "activation", sg, bg, AF.Sigmoid)
                self.op("dve", "tensor_tensor", M[c], sg, bp, ALU.mult)
        self.ws.release(f"L{l}.p.wp")
        rstd = self.rms_stats(M, SQ, "ones_mean", "eps_rms")
        self.add_to_h(M, l, 7, rstd)

    def gmlp(self, l):
        j = l // 2
        XN = Role(self, 0, 8, BF16).chunks(8, TT)
        SQ = Role(self, 8, 8, BF16).chunks(8, TT)
        UTr = Role(self, 16, 16, BF16)
        UT = UTr.chunks(16, TT)
        M = Role(self, 38, 16, F32).chunks(8, TT)
        VTOK = Role(self, 54, 32, F32).chunks(4, 2048)
        VN = Role(self, 86, 8, BF16).chunks(2, 2048)
        rstd = self.rms_stats(self.Hc, SQ, "ones_mean", "eps_rms")
        self.norm_to(self.Hc, XN, l, 2, rstd)
        for q in range(4):
            w = self.ws.get(f"L{l}.g.wu.{q}").re("p (k c) -> p k c", k=8, c=512)
            for c4 in range(4):
                ec = 4 * q + c4
                b = self.bank()
                self.mm(b, [(w[:, k, c4 * 128:(c4 + 1) * 128], XN[k]) for k in range(NCH)])
                self.op("act", "activation", UT[ec], b, AF.Gelu, bias=self.pc(f"binu{j}", ec), scale=1.0)
        rows = self.ws.get(f"L{l}.g.rows", hold=True)
        ones_row = self.cm("ones1", 0, 1, 0, 128)
        for es in range(4):
            w = self.ws.get(f"L{l}.g.wv.{es}").re("p (k c) -> p k c", k=8, c=512)
            for tb in range(4):
                b = self.bank()
                terms = [(XN[k][:, tb * 128:(tb + 1) * 128], w[:, k, :]) for k in range(NCH)]
                terms.append((ones_row, rows[0:1, es * 512:(es + 1) * 512]))
                self.mm(b, terms)
                self.op("act", "activation", VTOK[tb][:, es * 512:(es + 1) * 512], b, AF.Gelu)
        vg = self.ws.get(f"L{l}.g.vg", hold=True)
        wsb = self.ws.get(f"L{l}.g.ws", hold=True)
        wsv = wsb[:, 0:1024].re("p (g i) -> p g i", g=8, i=128)
        self.op("dve", "memset", wsv[64:128, :, 0:64], 0.0)
        sm = self.small
        for tb in range(4):
            for c in range(4):
                self.op("dve", "bn_stats", sm[:, c * 6:(c + 1) * 6], VTOK[tb][:, c * 512:(c + 1) * 512])
            self.op("dve", "bn_aggr", sm[:, 24:26], sm[:, 0:24].re("p (c d) -> p c d", d=6))
            self.op("act", "activation", sm[:, 26:27], sm[:, 25:26], AF.Sqrt, bias=self.pc("eps_ln"), scale=1.0)
            self.op("dve", "reciprocal", sm[:, 26:27], sm[:, 26:27])
            self.op("dve", "scalar_tensor_tensor", sm[:, 27:28], sm[:, 24:25], -1.0, sm[:, 26:27], ALU.mult, ALU.mult)
            self.op("act", "activation", VTOK[tb], VTOK[tb], AF.Identity, bias=sm[:, 27:28], scale=sm[:, 26:27])
            vn = VN[tb % 2]
            self.op("dve", "tensor_tensor", vn, VTOK[tb], vg[:, 0:2048], ALU.mult)
            for q in range(4):
                b = self.bank()
                for c4 in range(4):
                    ec = 4 * q + c4
                    gi = ec // 2
                    bo = b[:, c4 * 128:(c4 + 1) * 128]
                    self.mm(bo, [(vn[:, ec * 128:(ec + 1) * 128], wsv[:, gi, :]),
                                 (ones_row, rows[0:1, 2048 + gi * 128:2048 + (gi + 1) * 128])])
                uv = V(UTr.ap.rearrange("p (e t) -> p e t", e=16, t=TT)[:, 4 * q:4 * q + 4, tb * 128:(tb + 1) * 128],
                       [UT[4 * q + c4].ts[0] for c4 in range(4)])
                self.op("dve", "tensor_tensor", uv, b.re("p (c i) -> p c i", c=4, i=128), uv, ALU.mult)
        for nm in ("rows", "vg", "ws"):
            self.ws.release(f"L{l}.g.{nm}")
        for q in range(4):
            w = self.ws.get(f"L{l}.g.wo.{q}").re("p (e c) -> p e c", e=16, c=256)
            for c2 in range(2):
                c = 2 * q + c2
                b = self.bank()
                self.mm(b, [(w[:, ec, c2 * 128:(c2 + 1) * 128], UT[ec]) for ec in range(16)])
                self.op("act", "copy", M[c], b)
        rstd = self.rms_stats(M, SQ, "ones_mean", "eps_rms")
        self.add_to_h(M, l, 3, rstd)


    def rwkv(self, l, t):
        for s in range(2):
            self.rwkv_sub(l, t, s)

    def rwkv_sub(self, l, t, s):
        j = l // 2
        TR = 256
        c0 = s * TR
        R = lambda u0, nu, dt=BF16: Role(self, u0, nu, dt)
        X, DX = R(0, 4), R(4, 4)
        XM = [R(8, 4), R(12, 4)]
        rK, rKK, rA, rR, rV, SQ = R(16, 4), R(20, 4), R(24, 4), R(28, 4), R(32, 4), R(36, 4)
        F3, F4 = R(40, 8, F32), R(48, 8, F32)
        AR, BK = R(56, 8), R(64, 8)
        ET = R(72, 3, F32).chunks(3, TR)
        A_ = [R(75, 2), R(77, 2)]
        N_ = [R(79, 2), R(81, 2)]
        T_ = [R(83, 2), R(85, 2)]
        GM = [R(91, 4), R(95, 4)]
        UV = [R(99, 2), R(101, 2)]
        BKT = [R(103, 2), R(105, 2)]
        RHST = R(107, 2)
        VT = [R(109, 2), R(111, 2)]
        SN = R(113, 2)
        TF = [R(87, 2), R(89, 2)]
        W1, W2 = R(75, 8, F32), R(91, 8, F32)
        Xc, DXc = X.chunks(8, TR), DX.chunks(8, TR)
        XMc = [XM[0].chunks(8, TR), XM[1].chunks(8, TR)]
        rKc, rKKc, rAc, rRc, rVc, SQc = (r.chunks(8, TR) for r in (rK, rKK, rA, rR, rV, SQ))
        F3c, F4c = F3.chunks(8, TR), F4.chunks(8, TR)
        b3 = lambda role: role.v().re("p (k t) -> p k t", k=8, t=TR)
        b4 = lambda role: role.v().re("p (k c x) -> p k c x", k=8, c=4, x=64)
        AR4 = AR.v().re("p (k c x) -> p k c x", k=8, c=4, x=128)
        BK4 = BK.v().re("p (k c x) -> p k c x", k=8, c=4, x=128)
        AR3 = [AR.v(c * 512, (c + 1) * 512).re("p (tc x) -> p tc x", tc=4, x=128) for c in range(8)]
        BK3 = [BK.v(c * 512, (c + 1) * 512).re("p (tc x) -> p tc x", tc=4, x=128) for c in range(8)]
        Hs = [self.Hc[k][:, c0:c0 + TR] for k in range(NCH)]
        ident = self.cm("ident")
        blk1 = self.cm("blk_1")
        blkg = self.cm("blk_gn")

        def pb(name):
            c = self.p1c[name]
            return self.P1[:, c:c + 8].re("p (k o) -> p k o", k=8, o=1).bc([128, 8, TR])

        def psum_wide(n):
            while self.bank_i % n:
                self.bank_i = (self.bank_i + 1) % 8
            i = self.bank_i
            self.bank_i = (self.bank_i + n) % 8
            ts = []
            for q in range(n):
                ts += self.banks[i + q].ts
            return V(self.psum[:, i * 512:(i + n) * 512], ts), [self.banks[i + q] for q in range(n)]

        rstd = self.rms_stats(Hs, SQc, "ones_mean", "eps_rms", n=TR)
        self.norm_to(Hs, Xc, l, 2, rstd)
        X3, DX3 = b3(X), b3(DX)
        xp3 = self.xprev[j].re("p (k o) -> p k o", k=8, o=1)
        self.op("dve", "tensor_tensor", DX3[:, :, 1:TR], X3[:, :, 0:TR - 1], X3[:, :, 1:TR], ALU.subtract)
        self.op("dve", "tensor_tensor", DX3[:, :, 0:1], xp3, X3[:, :, 0:1], ALU.subtract)
        self.op("act", "copy", xp3, X3[:, :, TR - 1:TR])

        def mix(m, dst):
            for k in range(NCH):
                self.op("dve", "scalar_tensor_tensor", dst[k], DXc[k], self.pc(f"mu{j}", m * 8 + k), Xc[k],
                        ALU.mult, ALU.add)
            return dst

        la = self.ws.get(f"L{l}.r.la.{s}", hold=True)
        lb = self.ws.get(f"L{l}.r.lb.{s}", hold=True)
        w1 = la[:, 0:512].re("p (k c) -> p k c", k=8, c=64)
        a1 = la[:, 512:1024].re("p (k c) -> p k c", k=8, c=64)
        g1 = la[:, 1024:2304].re("p (k c) -> p k c", k=8, c=160)
        v1 = la[:, 2304:2560].re("p (k c) -> p k c", k=8, c=32)
        w2, a2, v2 = lb[:, 0:1024], lb[:, 1024:2048], lb[:, 2048:3072]

        def proj(n, xm, outc):
            for q in range(2):
                w = self.ws.get(f"L{l}.r.win{n}.{q}.{s}").re("p (k c) -> p k c", k=8, c=512)
                for c4 in range(4):
                    c = 4 * q + c4
                    bb = self.bank()
                    self.mm(bb[:, 0:TR], [(w[:, k, c4 * 128:(c4 + 1) * 128], xm[k]) for k in range(NCH)])
                    self.op("act", "copy", outc[c], bb[:, 0:TR])

        xw = mix(3, XMc[0])
        b = self.bank()
        self.mm(b[0:64, 0:TR], [(w1[:, k, :], xw[k]) for k in range(NCH)])
        self.op("act", "activation", self.TW[0:64, 0:TR], b[0:64, 0:TR], AF.Tanh)
        pw, _ = psum_wide(4)
        for c in range(8):
            self.mm(pw[:, c * TR:(c + 1) * TR], [(w2[0:64, c * 128:(c + 1) * 128], self.TW[0:64, 0:TR])])
        for c in range(8):
            self.op("act", "activation", F3c[c], pw[:, c * TR:(c + 1) * TR], AF.Sigmoid, bias=self.pc(f"w0{j}", c),
                    scale=1.0)
        xa = mix(4, XMc[1])
        b = self.bank()
        self.mm(b[0:64, 0:TR], [(a1[:, k, :], xa[k]) for k in range(NCH)])
        self.op("act", "copy", self.TA[0:64, 0:TR], b[0:64, 0:TR])
        pw, _ = psum_wide(4)
        for c in range(8):
            self.mm(pw[:, c * TR:(c + 1) * TR], [(a2[0:64, c * 128:(c + 1) * 128], self.TA[0:64, 0:TR])])
        for c in range(8):
            self.op("act", "activation", rAc[c], pw[:, c * TR:(c + 1) * TR], AF.Sigmoid, bias=self.pc(f"a0{j}", c),
                    scale=1.0)
        xk = mix(1, XMc[0])
        proj(1, xk, rKc)
        src = F3.v().re("p (g t) -> p g t", t=64)
        dst = F4.v().re("p (g t) -> p g t", t=64)
        for sft in (1, 2, 4, 8, 16, 32):
            self.op("dve", "tensor_tensor", dst[:, :, sft:64], src[:, :, sft:64], src[:, :, 0:64 - sft], ALU.add)
            self.op("act", "copy", dst[:, :, 0:sft], src[:, :, 0:sft])
            src, dst = dst, src
        self.op("act", "activation", F4.v(), F3.v(), AF.Exp, scale=-C0)
        self.op("act", "activation", F3.v(), F3.v(), AF.Exp, scale=C0)
        PC3 = self.PC.re("p (k c) -> p k c", k=8, c=4)
        self.op("dve", "tensor_copy", PC3, F4.v().re("p (k c t) -> p k c t", k=8, c=4, t=64)[:, :, :, 63])
        PCX = self.PCX.re("p (k e c) -> p k e c", k=8, e=2, c=4)
        self.op("act", "copy", PCX[0:64, :, 0, :], PC3[0:64, :, :])
        self.op("dve", "tensor_copy", PCX[0:64, :, 1, :], PC3[64:128, :, :])

        xr = mix(0, XMc[1])
        proj(0, xr, rRc)
        xv = mix(2, XMc[0])
        proj(2, xv, rVc)
        if j >= 1:
            b = self.bank()
            self.mm(b[0:32, 0:TR], [(v1[:, k, :], xv[k]) for k in range(NCH)])
            self.op("act", "copy", self.TV[0:32, 0:TR], b[0:32, 0:TR])

        K3, KK3, A3, R3, V3, SQ3 = b3(rK), b3(rKK), b3(rA), b3(rR), b3(rV), b3(SQ)
        E1_4, E3_4 = b4(F4), b4(F3)
        w1v, w2v = W1.v(), W2.v()
        w1_3 = w1v.re("p (k t) -> p k t", k=8, t=TR)
        w2_3 = w2v.re("p (k t) -> p k t", k=8, t=TR)
        w1_4 = w1v.re("p (k c x) -> p k c x", k=8, c=4, x=64)
        self.op("dve", "tensor_tensor", KK3, K3, pb(f"kk{j}"), ALU.mult)
        self.op("act", "activation", SQ3, KK3, AF.Square)
        pw, _ = psum_wide(4)
        for c in range(8):
            self.mm(pw[:, c * TR:(c + 1) * TR], [(blk1, SQc[c])])
        self.op("dve", "tensor_scalar_max", w1v, pw, 1e-24)
        self.op("act", "activation", w1v, w1v, AF.Ln)
        self.op("act", "activation", w1v, w1v, AF.Exp, scale=-0.5)
        self.op("dve", "tensor_tensor", KK3, KK3, w1_3, ALU.mult)
        self.op("dve", "tensor_tensor", w2_3, A3, pb(f"ka{j}"), ALU.mult)
        self.op("dve", "tensor_tensor", w2_3, w2_3, pb(f"omka{j}"), ALU.add)
        self.op("dve", "tensor_tensor", K3, K3, w2_3, ALU.mult)
        self.op("dve", "tensor_tensor", w1_3, KK3, A3, ALU.mult)
        self.op("pool", "tensor_tensor", BK4[:, :, :, 0:64], w1_4, E3_4, ALU.mult)
        self.op("pool", "tensor_tensor", BK4[:, :, :, 64:128], b4(rK), E3_4, ALU.mult)
        self.op("dve", "scalar_tensor_tensor", AR4[:, :, :, 1:64], b4(rKK)[:, :, :, 1:64], -1.0,
                E1_4[:, :, :, 0:63], ALU.mult, ALU.mult)
        self.op("act", "mul", AR4[:, :, :, 0:1], b4(rKK)[:, :, :, 0:1], -1.0)
        self.op("pool", "tensor_tensor", AR4[:, :, :, 64:128], b4(rR), E1_4, ALU.mult)
        self.op("dve", "tensor_tensor", SQ3, R3, pb(f"rk{j}"), ALU.mult)
        self.op("dve", "tensor_tensor", SQ3, SQ3, K3, ALU.mult)
        VFs = V(self.VF.ap.rearrange("p (k t) -> p k t", k=8, t=TT)[:, :, c0:c0 + TR], [c.ts[0] for c in self.VFc])
        if j >= 1:
            pw, _ = psum_wide(4)
            for c in range(8):
                self.mm(pw[:, c * TR:(c + 1) * TR], [(v2[0:32, c * 128:(c + 1) * 128], self.TV[0:32, 0:TR])])
            for c in range(8):
                self.op("act", "activation", w2v[:, c * TR:(c + 1) * TR], pw[:, c * TR:(c + 1) * TR], AF.Sigmoid,
                        bias=self.pc(f"v0{j}", c), scale=1.0)
            self.op("dve", "tensor_tensor", w1_3, VFs, V3, ALU.subtract)
            self.op("dve", "tensor_tensor", w1_3, w1_3, w2_3, ALU.mult)
            self.op("dve", "tensor_tensor", V3, V3, w1_3, ALU.add)
        else:
            self.op("pool", "tensor_copy", VFs, V3)
        pw, _ = psum_wide(4)
        for c in range(8):
            self.mm(pw[:, c * TR:(c + 1) * TR], [(blk1, SQc[c])])
        self.op("dve", "tensor_tensor", A3, pw.re("p (k t) -> p k t", k=8, t=TR), V3, ALU.mult)

        xg = mix(5, XMc[1])
        lc = self.ws.get(f"L{l}.r.lc.{s}", hold=True)
        g2a, g2b = lc[:, 0:1024], lc[:, 1024:2048]
        b1 = self.bank()
        self.mm(b1[:, 0:TR], [(g1[:, k, 0:128], xg[k]) for k in range(NCH)])
        b2 = self.bank()
        self.mm(b2[0:32, 0:TR], [(g1[:, k, 128:160], xg[k]) for k in range(NCH)])
        self.op("act", "activation", self.TG1[:, 0:TR], b1[:, 0:TR], AF.Sigmoid)
        self.op("act", "activation", self.TG2[0:32, 0:TR], b2[0:32, 0:TR], AF.Sigmoid)
        pw, _ = psum_wide(4)
        for c in range(8):
            self.mm(pw[:, c * TR:(c + 1) * TR], [(g2a[:, c * 128:(c + 1) * 128], self.TG1[:, 0:TR]),
                                                 (g2b[0:32, c * 128:(c + 1) * 128], self.TG2[0:32, 0:TR])])
        self.op("act", "copy", rKK.v(), pw)
        for nm in ("la", "lb", "lc"):
            self.ws.release(f"L{l}.r.{nm}.{s}")

        ST4 = self.ST[j].re("p (k h i) -> p k h i", k=8, h=2, i=64)
        Y4 = F4.v().re("p (k c t) -> p k c t", k=8, c=4, t=64)
        maskg = self.cm("maskg4", 0, 128, 0, 512)
        maskn = self.cm("maskn16", 0, 64, 0, 1024)
        id16 = self.cm("ident16", 0, 64, 0, 1024)
        for st in range(2):
            self.op("dve", "memset", VT[st].v()[0:64, :], 0.0)

        def hs_(h):
            return slice(h * 64, (h + 1) * 64)

        def tphase(tc):
            st = tc % 2
            gm4 = GM[st].v().re("p (d e x) -> p d e x", d=8, e=2, x=128)
            gm3 = GM[st].v().re("p (h x) -> p h x", h=16, x=128)
            for hf in range(2):
                bt = self.bank()
                for d4 in range(4):
                    dc = 4 * hf + d4
                    self.op("pe", "matmul", bt[:, d4 * 128:(d4 + 1) * 128], BK3[dc][:, tc, :], ident,
                            start=True, stop=True, inc=(d4 == 3))
                self.op("act", "copy", BKT[st].v()[:, hf * 512:(hf + 1) * 512], bt)
            for hf in range(2):
                bv = self.bank()
                for d4 in range(4):
                    dc = 4 * hf + d4
                    self.op("pe", "matmul", bv[0:64, d4 * 128:(d4 + 1) * 128], rVc[dc][:, tc * 64:(tc + 1) * 64], ident,
                            start=True, stop=True, inc=(d4 == 3))
                self.op("dve", "tensor_copy", VT[st].v()[64:128, hf * 512:(hf + 1) * 512], bv[0:64, :])
            yield
            for hp in range(2):
                ps = slice(64 * hp, 64 * hp + 64)
                for q in range(2):
                    bg = self.bank()
                    for d4 in range(4):
                        dc = 4 * q + d4
                        self.op("pe", "matmul", bg[:, d4 * 128:(d4 + 1) * 128], BK3[dc][ps, tc, :],
                                AR3[dc][ps, tc, :], start=True, stop=True, inc=(d4 == 3))
                    self.op("dve", "tensor_tensor", gm4[:, 4 * q:4 * q + 4, hp, :],
                            bg.re("p (d x) -> p d x", d=4, x=128),
                            maskg.re("p (d x) -> p d x", d=4, x=128), ALU.mult)
            n4 = N_[0].v()[0:64, :].re("p (d e x) -> p d e x", d=8, e=2, x=64)
            for hp in range(2):
                ps = slice(64 * hp, 64 * hp + 64)
                bn1 = self.bank()
                for dc in range(8):
                    self.op("pe", "matmul", bn1[0:64, dc * 64:(dc + 1) * 64],
                            AR3[dc][ps, tc, 0:64], BK3[dc][ps, tc, 0:64], start=True, stop=True, inc=(dc == 7))
                self.op("dve", "tensor_tensor", n4[:, :, hp, :], bn1[0:64, :].re("p (d x) -> p d x", d=8, x=64),
                        maskn[:, 0:512].re("p (d x) -> p d x", d=8, x=64), ALU.mult)
            self.op("act", "copy", A_[0].v()[0:64, :].re("p (h x) -> p h x", h=16, x=64), gm3[0:64, :, 0:64])
            self.op("dve", "tensor_tensor", T_[0].v()[0:64, :], A_[0].v()[0:64, :], id16, ALU.add)
            yield
            cur, tcur = 0, 0
            for step in range(1, 7):
                nxt = 1 - cur
                Ac, Nc = A_[cur].v()[0:64, :], N_[cur].v()[0:64, :]
                An, Nn = A_[nxt].v()[0:64, :], N_[nxt].v()[0:64, :]
                Tc, Tn = T_[tcur].v()[0:64, :], T_[1 - tcur].v()[0:64, :]
                if step == 6:
                    Tn = TF[st].v()[0:64, :]
                do_sq = step <= 5
                do_a = step <= 4
                do_t = step >= 2
                if do_a:
                    ba = [self.bank(), self.bank()]
                    for h in range(16):
                        self.op("pe", "matmul", ba[h // 8][0:64, hs_(h % 8)], Nc[:, hs_(h)], Ac[:, hs_(h)],
                                start=True, stop=True, inc=(h % 8 == 7))
                if do_sq:
                    bn = [self.bank(), self.bank()]
                    for h in range(16):
                        self.op("pe", "matmul", bn[h // 8][0:64, hs_(h % 8)], Ac[:, hs_(h)], Nc[:, hs_(h)],
                                start=True, stop=True, inc=(h % 8 == 7))
                if do_t:
                    bt2 = [self.bank(), self.bank()]
                    for h in range(16):
                        self.op("pe", "matmul", bt2[h // 8][0:64, hs_(h % 8)], Nc[:, hs_(h)], Tc[:, hs_(h)],
                                start=True, stop=True, inc=(h % 8 == 7))
                for hf in range(2):
                    fs = slice(hf * 512, (hf + 1) * 512)
                    if do_a:
                        self.op("act", "copy", An[:, fs], ba[hf][0:64, :])
                    if do_sq:
                        self.op("act" if not do_a else "dve", "copy" if not do_a else "tensor_copy", Nn[:, fs],
                                bn[hf][0:64, :])
                    if do_t:
                        self.op("dve", "tensor_tensor", Tn[:, fs], bt2[hf][0:64, :], Tc[:, fs], ALU.add)
                if do_sq:
                    cur = nxt
                if do_t:
                    tcur = 1 - tcur
                yield

        def chain(tc):
            st = tc % 2
            gm3 = GM[st].v().re("p (h x) -> p h x", h=16, x=128)
            uv3 = UV[st].v().re("p (h i) -> p h i", h=16, i=64)
            bkt3 = BKT[st].v().re("p (h i) -> p h i", h=16, i=64)
            vt3 = VT[st].v().re("p (h i) -> p h i", h=16, i=64)
            Tf = TF[st].v()[0:64, :]
            br = [self.bank(), self.bank()]
            for h in range(16):
                dc, hp = h // 2, h % 2
                out = br[h // 8][0:64, hs_(h % 8)]
                self.op("pe", "matmul", out, AR3[dc][:, tc, 0:64], ST4[:, dc, hp, :], start=True, stop=False, inc=False)
                self.op("pe", "matmul", out, gm3[:, h, 0:64], vt3[:, h, :], start=False, stop=True,
                        inc=(h % 8 == 7))
            self.op("act", "copy", RHST.v()[0:64, 0:512], br[0][0:64, :])
            self.op("dve", "tensor_copy", RHST.v()[0:64, 512:1024], br[1][0:64, :])
            yield
            bu = [self.bank(), self.bank()]
            for h in range(16):
                self.op("pe", "matmul", bu[h // 8][0:64, hs_(h % 8)], Tf[:, hs_(h)],
                        RHST.v()[0:64, hs_(h)], start=True, stop=True, inc=(h % 8 == 7))
            self.op("act", "copy", UV[st].v()[0:64, 0:512], bu[0][0:64, :])
            self.op("dve", "tensor_copy", UV[st].v()[0:64, 512:1024], bu[1][0:64, :])
            self.op("pool", "tensor_copy", UV[st].v()[64:128, :], VT[st].v()[64:128, :])
            yield
            bs = [self.bank(), self.bank()]
            for h in range(16):
                dc, hp = h // 2, h % 2
                out = bs[h // 8][0:64, hs_(h % 8)]
                self.op("pe", "matmul", out, bkt3[:, h, :], uv3[:, h, :], start=True, stop=False, inc=False)
                self.op("pe", "matmul", out, ident[:, 64 * hp:64 * hp + 64], ST4[:, dc, hp, :], start=False, stop=True,
                        inc=(h % 8 == 7))
            by = [self.bank(), self.bank()]
            for h in range(16):
                dc, hp = h // 2, h % 2
                out = by[h // 8][0:64, hs_(h % 8)]
                self.op("pe", "matmul", out, ST4[:, dc, hp, :], AR3[dc][:, tc, 64:128], start=True, stop=False,
                        inc=False)
                self.op("pe", "matmul", out, uv3[:, h, :], gm3[:, h, 64:128], start=False, stop=True,
                        inc=(h % 8 == 7))
            sn = SN.v()[0:64, :]
            for hf in range(2):
                self.op("dve", "tensor_tensor", sn[:, hf * 512:(hf + 1) * 512].re("p (d e i) -> p d e i", d=4, e=2, i=64),
                        bs[hf][0:64, :].re("p (d e i) -> p d e i", d=4, e=2, i=64),
                        PCX[0:64, 4 * hf:4 * hf + 4, :, tc:tc + 1].bc([64, 4, 2, 64]), ALU.mult)
            sn4 = sn.re("p (d e i) -> p d e i", d=8, e=2, i=64)
            self.op("act", "copy", ST4[0:64, :, 0, :], sn4[:, :, 0, :])
            self.op("dve", "tensor_copy", ST4[64:128, :, 1, :], sn4[:, :, 1, :])
            for hf in range(2):
                byv = by[hf][0:64, :].re("p (d e t) -> p d e t", d=4, e=2, t=64)
                self.op("act", "copy", Y4[0:64, 4 * hf:4 * hf + 4, tc, :], byv[:, :, 0, :])
                self.op("dve", "tensor_copy", Y4[64:128, 4 * hf:4 * hf + 4, tc, :], byv[:, :, 1, :])
            yield

        def drain(gen):
            for _ in gen:
                pass

        drain(tphase(0))
        for tc in range(4):
            gc = chain(tc)
            gt = tphase(tc + 1) if tc + 1 < 4 else iter(())
            alive_c, alive_t = True, True
            while alive_c or alive_t:
                if alive_t:
                    try:
                        next(gt)
                    except StopIteration:
                        alive_t = False
                if alive_t:
                    try:
                        next(gt)
                    except StopIteration:
                        alive_t = False
                if alive_c:
                    try:
                        next(gc)
                    except StopIteration:
                        alive_c = False

        Y3 = b3(F4)
        xo = XMc[0]
        self.op("act", "copy", SQ3, Y3)
        pw, _ = psum_wide(4)
        for c in range(8):
            self.mm(pw[:, c * TR:(c + 1) * TR], [(blkg, SQc[c])])
        self.op("dve", "tensor_tensor", Y3, Y3, pw.re("p (k t) -> p k t", k=8, t=TR), ALU.subtract)
        self.op("act", "activation", SQ3, Y3, AF.Square)
        pw, _ = psum_wide(4)
        for c in range(8):
            self.mm(pw[:, c * TR:(c + 1) * TR], [(blkg, SQc[c])])
        self.op("act", "activation", w1v, pw, AF.Ln, bias=self.pc("eps_gn"), scale=1.0)
        self.op("act", "activation", w1v, w1v, AF.Exp, scale=-0.5)
        self.op("dve", "tensor_tensor", Y3, Y3, w1_3, ALU.mult)
        self.op("dve", "tensor_tensor", Y3, Y3, pb(f"ln0{j}"), ALU.mult)
        self.op("pool", "tensor_tensor", Y3, Y3, pb(f"ln1{j}"), ALU.add)
        self.op("pool", "tensor_tensor", Y3, Y3, A3, ALU.add)
        self.op("dve", "tensor_tensor", b3(XM[0]), Y3, KK3, ALU.mult)
        Mc = F3c
        for q in range(2):
            w = self.ws.get(f"L{l}.r.wo.{q}.{s}").re("p (k c) -> p k c", k=8, c=512)
            for c4 in range(4):
                c = 4 * q + c4
                bb = self.bank()
                self.mm(bb[:, 0:TR], [(w[:, k, c4 * 128:(c4 + 1) * 128], xo[k]) for k in range(NCH)])
                self.op("act", "copy", Mc[c], bb[:, 0:TR])
        rstd = self.rms_stats(Mc, SQc, "ones_mean", "eps_rms", n=TR)
        self.add_to_h(Mc, l, 3, rstd, hsl=(c0, c0 + TR))

    def load_x(self, t):
        self.fw.dma("sp", "io_x", self.H.ap.rearrange("p (k t) -> p k t", k=NCH),
                    self.xT[:, :, t * TT:(t + 1) * TT], writes=self.H.ts)

    def store(self, t):
        self.fw.dma("sp", "io_y", self.yT[:, :, t * TT:(t + 1) * TT],
                    self.H.ap.rearrange("p (k t) -> p k t", k=NCH), reads=self.H.ts)

    def run(self, stages=None):
        self.setup()
        for t in range(self.ntiles):
            self.load_x(t)
            for l in range(self.nlayers):
                self.ffn(l, 0)
                if l % 2 == 0:
                    self.gmlp(l)
                else:
                    self.rwkv(l, t)
                self.ffn(l, 1)
                self.ple(l, t)
            self.store(t)
        self.fw.wait_all("sp", self.H.ts)
        self.fw.emit()


def build_nc(blocks_meta, ntiles, nlayers, s_core):
    nc = bass.Bass("TRN2", target_bir_lowering=False)
    with ExitStack() as st:
        g = Gen(nc, st, blocks_meta, ntiles, nlayers, s_core)
        g.run()
        n_ins = g.fw.n_ins
    return nc, n_ins


def prep_shared(inp, nlayers=4):
    blocks = block_list(inp, nlayers)
    meta = [(n, a.shape[1]) for n, a in blocks]
    W = np.concatenate([a for _, a in blocks], axis=1)
    return meta, W, build_p1(inp), build_cst()


def prep_core(x_b, p_b):
    S = x_b.shape[0]
    xT = np.ascontiguousarray(x_b.reshape(S, NCH, 128).transpose(2, 1, 0))
    pT = np.ascontiguousarray(p_b.reshape(4, S, 2, 128).transpose(3, 0, 2, 1))
    return xT, pT


def kernel(**inputs):
    inp = {k: np.asarray(v, np.float32) for k, v in inputs.items()}
    meta, W, P1, CSTa = prep_shared(inp)
    B = inp["x"].shape[0]
    nc, _ = build_nc(meta, SEQ // TT, 4, SEQ)
    in_maps = []
    for b in range(B):
        xT, pT = prep_core(inp["x"][b], inp["p"][:, b])
        in_maps.append({"xT": xT, "pT": pT, "W": W, "P1": P1, "CST": CSTa})
    res = run_bass_kernel_spmd(nc, in_maps, core_ids=list(range(B)))
    out = np.empty((B, SEQ, D), np.float32)
    for b in range(B):
        yT = res.results[b]["yT"]
        out[b] = yT.transpose(2, 1, 0).reshape(SEQ, D)
    return out
```
